# Optimizing a Trainium2 kernel written in Bass

```python
import math
import jax, jax.numpy as jnp
from jax import lax
import numpy as np

D_MODEL = 1024
BATCH = 8
SEQ = 8192
DEPTH = 1

GLA_HEADS = 4
GLA_DK = 64
GLA_DV = 128
GLA_GATE_RANK = 16
GLA_TAU = 16.0
GLA_CHUNK = 64
DSA_HEADS = 8
DSA_HEAD_DIM = 64
IDX_HEADS = 8
IDX_DIM = 32
DSA_TOPK_MAX = 256
DSA_QBLOCK = 128
ROPE_THETA = 500000.0
ROPE_FRACTION = 4
N_BRANCH = 2
BRANCH_WIDTH = 512
D_FF = 2816
CONV_WIDTH = 3
LN_EPS = 1e-5
RMS_EPS = 1e-6
DEEPNORM_ALPHA = (2.0 * DEPTH) ** 0.25
DEEPNORM_BETA = (8.0 * DEPTH) ** -0.25

IN_WIDTHS = (
    GLA_HEADS * GLA_DK,
    GLA_HEADS * GLA_DK,
    GLA_HEADS * GLA_DV,
    GLA_GATE_RANK,
    GLA_HEADS * GLA_DV,
    DSA_HEADS * DSA_HEAD_DIM,
    DSA_HEAD_DIM,
    DSA_HEAD_DIM,
    IDX_HEADS * IDX_DIM,
    IDX_DIM,
    IDX_HEADS,
    N_BRANCH * D_MODEL,
)
D_IN = sum(IN_WIDTHS)

kernel_name = "hybrid_gla_dsa_gated_merge_deepnorm"


def split_cols(h, widths):
    outs = []
    off = 0
    for w in widths:
        outs.append(h[..., off:off + w])
        off += w
    return outs


def layer_norm(x, gain, bias):
    xf = x.astype(jnp.float32)
    mu = jnp.mean(xf, axis=-1, keepdims=True)
    var = jnp.mean(jnp.square(xf - mu), axis=-1, keepdims=True)
    y = (xf - mu) * lax.rsqrt(var + LN_EPS) * gain.astype(jnp.float32) + bias.astype(jnp.float32)
    return y.astype(x.dtype)


def rope_partial(x, pos):
    d = x.shape[-1]
    rot = d // ROPE_FRACTION
    half = rot // 2
    inv_freq = ROPE_THETA ** (-jnp.arange(half, dtype=jnp.float32) * 2.0 / rot)
    ang = pos.astype(jnp.float32)[:, None] * inv_freq[None, :]
    cos = jnp.cos(ang)[:, None, :]
    sin = jnp.sin(ang)[:, None, :]
    x1 = x[..., :half].astype(jnp.float32)
    x2 = x[..., half:rot].astype(jnp.float32)
    out = jnp.concatenate([(x1 * cos - x2 * sin).astype(x.dtype),
                           (x2 * cos + x1 * sin).astype(x.dtype),
                           x[..., rot:]], axis=-1)
    return out


def gla_mixer(q, k, v, a_low, r, w_a2, b_a, norm_gain):
    B, L, _ = q.shape
    dt = q.dtype
    f32 = jnp.float32
    q = q.reshape(B, L, GLA_HEADS, GLA_DK).astype(f32) * (GLA_DK ** -0.5)
    k = k.reshape(B, L, GLA_HEADS, GLA_DK).astype(f32)
    v = v.reshape(B, L, GLA_HEADS, GLA_DV).astype(f32)
    g = jax.nn.log_sigmoid((a_low @ w_a2 + b_a).astype(f32)) / GLA_TAU
    g = g.reshape(B, L, GLA_HEADS, GLA_DK)
    n_chunks = L // GLA_CHUNK

    def to_chunks(t):
        return t.reshape(B, n_chunks, GLA_CHUNK, GLA_HEADS, t.shape[-1]).swapaxes(0, 1)

    causal = jnp.tril(jnp.ones((GLA_CHUNK, GLA_CHUNK), dtype=bool))

    def step(state, inp):
        qc, kc, vc, gc = inp
        b = jnp.cumsum(gc, axis=1)
        b_last = b[:, -1]
        o_inter = jnp.einsum('bchk,bhkv->bchv', qc * jnp.exp(b), state)
        diff = b[:, :, None] - b[:, None, :]
        decay = jnp.exp(jnp.where(causal[None, :, :, None, None], diff, -jnp.inf))
        scores = jnp.einsum('bihk,bjhk,bijhk->bhij', qc, kc, decay)
        o_intra = jnp.einsum('bhij,bjhv->bihv', scores, vc)
        new_state = jnp.exp(b_last)[..., None] * state + jnp.einsum(
            'bjhk,bjhv->bhkv', kc * jnp.exp(b_last[:, None] - b), vc)
        return new_state, o_inter + o_intra

    state0 = jnp.zeros((B, GLA_HEADS, GLA_DK, GLA_DV), f32)
    _, o = lax.scan(step, state0, (to_chunks(q), to_chunks(k), to_chunks(v), to_chunks(g)))
    o = o.swapaxes(0, 1).reshape(B, L, GLA_HEADS, GLA_DV)
    o = o * lax.rsqrt(jnp.mean(jnp.square(o), axis=-1, keepdims=True) + RMS_EPS) * norm_gain.astype(f32)
    y = o.reshape(B, L, GLA_HEADS * GLA_DV) * jax.nn.silu(r.astype(f32))
    return y.astype(dt)


def dsa_mixer(q, k, v, qi, ki, wi, pos):
    B, L, _ = q.shape
    dt = q.dtype
    f32 = jnp.float32
    q = rope_partial(q.reshape(B, L, DSA_HEADS, DSA_HEAD_DIM), pos)
    k = rope_partial(k.reshape(B, L, 1, DSA_HEAD_DIM), pos)[:, :, 0]
    v = v.reshape(B, L, DSA_HEAD_DIM)
    qi = rope_partial(qi.reshape(B, L, IDX_HEADS, IDX_DIM), pos)
    ki = rope_partial(ki.reshape(B, L, 1, IDX_DIM), pos)[:, :, 0]
    wi = wi * (IDX_HEADS ** -0.5)
    top_k = min(DSA_TOPK_MAX, L // 4)
    n_blocks = L // DSA_QBLOCK
    key_pos = jnp.arange(L, dtype=jnp.int32)

    def blocks(t):
        return t.reshape((B, n_blocks, DSA_QBLOCK) + t.shape[2:]).swapaxes(0, 1)

    def gather_rows(table, idx):
        return table[idx]

    def one_block(args):
        q_b, qi_b, wi_b, t0 = args
        q_pos = t0 + jnp.arange(DSA_QBLOCK, dtype=jnp.int32)
        idx_logits = jnp.einsum('bqhd,bsd->bqhs', qi_b, ki) * (IDX_DIM ** -0.5)
        score = jnp.einsum('bqhs,bqh->bqs', jax.nn.relu(idx_logits), wi_b).astype(f32)
        admissible = key_pos[None, :] <= q_pos[:, None]
        score = jnp.where(admissible[None], score, -jnp.inf)
        _, sel = lax.top_k(score, top_k)
        k_sel = jax.vmap(gather_rows)(k, sel)
        v_sel = jax.vmap(gather_rows)(v, sel)
        valid = sel <= q_pos[None, :, None]
        logits = jnp.einsum('bqhd,bqkd->bqhk', q_b, k_sel).astype(f32) * (DSA_HEAD_DIM ** -0.5)
        logits = jnp.where(valid[:, :, None, :], logits, -jnp.inf)
        p = jax.nn.softmax(logits, axis=-1).astype(dt)
        return jnp.einsum('bqhk,bqkd->bqhd', p, v_sel)

    t0s = jnp.arange(n_blocks, dtype=jnp.int32) * DSA_QBLOCK
    o = lax.map(one_block, (blocks(q), blocks(qi), blocks(wi), t0s))
    return o.swapaxes(0, 1).reshape(B, L, DSA_HEADS * DSA_HEAD_DIM)


def causal_dwconv(u, w, b):
    L = u.shape[1]
    up = jnp.pad(u, ((0, 0), (CONV_WIDTH - 1, 0), (0, 0)))
    out = b
    for j in range(CONV_WIDTH):
        out = out + up[:, j:j + L] * w[j]
    return out


def setup_inputs(seed: int = 0) -> dict:
    key = jax.random.key(seed)
    ks = jax.random.split(key, 16)
    f32 = jnp.float32
    nrm = lambda k, shape, scale: jax.random.normal(k, shape, f32) * scale
    return {
        "x": jax.random.normal(ks[0], (BATCH, SEQ, D_MODEL), f32),
        "w_in": nrm(ks[1], (DEPTH, D_MODEL, D_IN), D_MODEL ** -0.5),
        "w_gla_a2": nrm(ks[2], (DEPTH, GLA_GATE_RANK, GLA_HEADS * GLA_DK), GLA_GATE_RANK ** -0.5),
        "b_gla_a": nrm(ks[3], (DEPTH, GLA_HEADS * GLA_DK), 0.1),
        "gla_norm_gain": 1.0 + nrm(ks[4], (DEPTH, GLA_HEADS, GLA_DV), 0.02),
        "w_branch": nrm(ks[5], (DEPTH, N_BRANCH, BRANCH_WIDTH, D_MODEL), BRANCH_WIDTH ** -0.5 * DEEPNORM_BETA),
        "w_o": nrm(ks[6], (DEPTH, D_MODEL, D_MODEL), D_MODEL ** -0.5 * DEEPNORM_BETA),
        "ln1_gain": 1.0 + nrm(ks[7], (DEPTH, D_MODEL), 0.02),
        "ln1_bias": nrm(ks[8], (DEPTH, D_MODEL), 0.02),
        "w_up": nrm(ks[9], (DEPTH, D_MODEL, 2 * D_FF), D_MODEL ** -0.5),
        "conv_w": nrm(ks[10], (DEPTH, CONV_WIDTH, 2 * D_FF), CONV_WIDTH ** -0.5),
        "conv_b": nrm(ks[11], (DEPTH, 2 * D_FF), 0.02),
        "w_down": nrm(ks[12], (DEPTH, D_FF, D_MODEL), D_FF ** -0.5 * DEEPNORM_BETA),
        "ln2_gain": 1.0 + nrm(ks[13], (DEPTH, D_MODEL), 0.02),
        "ln2_bias": nrm(ks[14], (DEPTH, D_MODEL), 0.02),
    }


def reference(x, w_in, w_gla_a2, b_gla_a, gla_norm_gain, w_branch, w_o, ln1_gain, ln1_bias,
              w_up, conv_w, conv_b, w_down, ln2_gain, ln2_bias):
    B, L, D = x.shape
    pos = jnp.arange(L, dtype=jnp.int32)
    for layer in range(DEPTH):
        h = x @ w_in[layer]
        (gq, gk, gv, ga, gr, dq, dk, dv, iq, ik, iw, gate) = split_cols(h, IN_WIDTHS)
        y_gla = gla_mixer(gq, gk, gv, ga, gr, w_gla_a2[layer], b_gla_a[layer], gla_norm_gain[layer])
        y_dsa = dsa_mixer(dq, dk, dv, iq, ik, iw, pos)
        branches = jnp.stack([y_gla, y_dsa], axis=2)
        proj = jnp.einsum('bsnc,ncd->bsnd', branches, w_branch[layer])
        gates = jax.nn.sigmoid(gate.reshape(B, L, N_BRANCH, D))
        mixed = jnp.sum(gates * proj, axis=2) @ w_o[layer]
        x = layer_norm(DEEPNORM_ALPHA * x + mixed, ln1_gain[layer], ln1_bias[layer])
        u = causal_dwconv(x @ w_up[layer], conv_w[layer], conv_b[layer])
        u_gate, u_val = u[..., :D_FF], u[..., D_FF:]
        f = (jax.nn.silu(u_gate) * u_val) @ w_down[layer]
        x = layer_norm(DEEPNORM_ALPHA * x + f, ln2_gain[layer], ln2_bias[layer])
    return x
```

```python
import math
from contextlib import ExitStack
import numpy as np
import concourse.bass as bass
import concourse.mybir as mybir
from concourse.bass_utils import run_bass_kernel_spmd

F32 = mybir.dt.float32
BF16 = mybir.dt.bfloat16
U8 = mybir.dt.uint8
ALU = mybir.AluOpType
ACTF = mybir.ActivationFunctionType
AX = mybir.AxisListType

D = 1024
KC = 8
D_FF = 2816
NFC = D_FF // 128
ALPHA = 2.0 ** 0.25
LN_EPS = 1e-5
RMS_EPS = 1e-6
NEG_BIG = -1.0e30
MASK_NEG = -30000.0

ENGINES = ("pe", "act", "dve", "pool", "sp")
SEM_CHUNK = 60000


class Buf:
    __slots__ = ("name", "last_w", "readers", "excl")

    def __init__(self, name, excl=False):
        self.name = name
        self.last_w = None
        self.readers = []
        self.excl = excl


class Op:
    __slots__ = ("eng", "fn", "deps", "signal", "dma_group", "dma_cum")

    def __init__(self, eng, fn):
        self.eng = eng
        self.fn = fn
        self.deps = []
        self.signal = False
        self.dma_group = None
        self.dma_cum = 0


class Prog:
    def __init__(self, nc, tag):
        self.nc = nc
        self.tag = tag
        self.ops = {e: [] for e in ENGINES}
        self.all_ops = []
        self.dma_groups = {}
        self.eng_obj = {"pe": nc.tensor, "act": nc.scalar, "dve": nc.vector, "pool": nc.gpsimd, "sp": nc.sync}
        self.finals = []

    def buf(self, name):
        return Buf(name)

    def op(self, eng, fn, reads=(), writes=()):
        o = Op(eng, fn)
        for b in reads:
            if b.last_w is not None:
                o.deps.append((b.last_w, 0))
            if b.excl:
                for r in b.readers:
                    if r.eng != eng:
                        o.deps.append((r, 0))
        for b in writes:
            if b.last_w is not None:
                o.deps.append((b.last_w, 0))
            for r in b.readers:
                o.deps.append((r, 1))
        for b in reads:
            b.readers.append(o)
        for b in writes:
            b.last_w = o
            b.readers = []
        self.ops[eng].append(o)
        self.all_ops.append(o)
        return o

    def dma(self, eng, out, in_, reads=(), writes=(), group=None, mode="seq", noncontig=False):
        if noncontig:
            def fn(e):
                with self.nc.allow_non_contiguous_dma(reason="tiny strided parameter load"):
                    return e.dma_start(out=out, in_=in_)
        else:
            def fn(e):
                return e.dma_start(out=out, in_=in_)
        o = self.op(eng, fn, reads, writes)
        g = self.dma_groups.setdefault(group, {"mode": mode, "count": 0})
        g["count"] += 1
        o.dma_group = group
        o.dma_cum = g["count"]
        return o

    def emit(self, E):
        nc = self.nc
        for o in self.all_ops:
            kept = []
            for (d, war) in o.deps:
                if d.dma_group is None and d.eng == o.eng and (war or o.eng == "pe"):
                    continue
                kept.append(d)
                d.signal = True
            o.deps = kept
        eng_sems = {}
        sig_count = {}
        for e in ENGINES:
            comp = [o for o in self.ops[e] if o.dma_group is None]
            if comp:
                comp[-1].signal = True
            n = 0
            for o in comp:
                if o.signal:
                    n += 1
                    sig_count[o] = n
            nsem = (n + SEM_CHUNK - 1) // SEM_CHUNK
            eng_sems[e] = [E(nc.semaphore(f"s{self.tag}_{e}_{k}")) for k in range(nsem)]
            if n:
                k, v = divmod(n - 1, SEM_CHUNK)
                self.finals.append((eng_sems[e][k], v + 1))
        dma_sems = {g: E(nc.semaphore(f"d{self.tag}_{g}")) for g in self.dma_groups}
        for g, info in self.dma_groups.items():
            self.finals.append((dma_sems[g], 16 * info["count"]))

        def target(d, o=None):
            if d.dma_group is not None:
                g = self.dma_groups[d.dma_group]
                assert not (g["mode"] == "all" and o is not None and o.dma_group == d.dma_group), "self-deadlock in 'all' dma group"
                cnt = g["count"] if g["mode"] == "all" else d.dma_cum
                return ("dma", d.dma_group), dma_sems[d.dma_group], 16 * cnt
            k, v = divmod(sig_count[d] - 1, SEM_CHUNK)
            return (d.eng, k), eng_sems[d.eng][k], v + 1

        n_wait = 0
        for e in ENGINES:
            eo = self.eng_obj[e]
            waited = {}
            eng_hi = {}
            for o in self.ops[e]:
                need = {}
                for d in o.deps:
                    key, sem, val = target(d, o)
                    if key[0] != "dma" and eng_hi.get(key[0], -1) > key[1]:
                        continue
                    if waited.get(key, 0) >= val:
                        continue
                    if key not in need or need[key][1] < val:
                        need[key] = (sem, val)
                for key, (sem, val) in need.items():
                    eo.wait_ge(sem, val)
                    n_wait += 1
                    waited[key] = val
                    if key[0] != "dma":
                        eng_hi[key[0]] = max(eng_hi.get(key[0], -1), key[1])
                ins = o.fn(eo)
                if o.dma_group is not None:
                    ins.then_inc(dma_sems[o.dma_group], 16)
                elif o.signal:
                    k, v = divmod(sig_count[o] - 1, SEM_CHUNK)
                    ins.then_inc(eng_sems[e][k], 1)
        self.n_wait = n_wait

    def barrier(self, engines=ENGINES):
        for e in engines:
            eo = self.eng_obj[e]
            for sem, val in self.finals:
                eo.wait_ge(sem, val)


class PsumPool:
    def __init__(self, nc, P, EP, n=8):
        self.free_list = []
        for k in range(n):
            t = EP(nc.psum_tensor(f"pb{P.tag}{k}", [128, 512], F32))
            self.free_list.append((t, Buf(f"pb{k}", excl=True)))

    def alloc(self):
        assert self.free_list, "PSUM pool exhausted"
        return self.free_list.pop(0)

    def free(self, item):
        self.free_list.append(item)


class Rot:
    def __init__(self, nc, P, EP, name, shape, dtype, n):
        self.items = [(EP(nc.sbuf_tensor(f"{name}{P.tag}{k}", shape, dtype)), P.buf(f"{name}{k}")) for k in range(n)]
        self.i = 0

    def next(self):
        it = self.items[self.i % len(self.items)]
        self.i += 1
        return it


def AP(t, off, pat):
    return bass.AP(t, off, [list(p) for p in pat])


_O = dict(gq=0, gk=256, gv=512, ga=1024, gr=1040, dq=1552, dk=2064, dv=2128, iq=2192, ik=2448, iw=2480, gate=2488)
_W = dict(gq=256, gk=256, gv=512, ga=16, gr=512, dq=512, dk=64, dv=64, iq=256, ik=32, iw=8, gate=2048)
_ORDER = ["gv", "gr", "dq", "gk", "iq", "dk", "dv", "ik", "iw", "ga", "gq", "gate"]
_C = {}
_off = 0
for _n in _ORDER:
    _C[_n] = _off
    _off += _W[_n]
W_IN_PERM = np.concatenate([np.arange(_O[n], _O[n] + _W[n]) for n in _ORDER])
NCOLA = _C["gate"]
MISC0 = _C["dk"]
MISCW = 184


def build(L, topk, nbis=12, debug=False, phases="ABC", stages=4, cut=99):
    NT = L // 128
    nc = bass.Bass("TRN2", target_bir_lowering=False)

    def din(name, shape, dt=F32):
        return nc.dram_tensor(name, list(shape), dt, kind="ExternalInput").ap()

    x_d = din("x", [L, D])
    w_in_d = din("w_in", [D, 4536])
    w_a2_d = din("w_a2", [16, 256])
    b_a_d = din("b_a", [1, 256])
    gain_d = din("gla_gain", [1, 512])
    w_br_d = din("w_br", [1024, D])
    w_o_d = din("w_o", [D, D])
    ln1g_d = din("ln1g", [1, D])
    ln1b_d = din("ln1b", [1, D])
    w_up_d = din("w_up", [D, 2 * D_FF])
    cw_d = din("conv_w", [3, 2 * D_FF])
    cb_d = din("conv_b", [1, 2 * D_FF])
    w_dn_d = din("w_down", [D_FF, D])
    ln2g_d = din("ln2g", [1, D])
    ln2b_d = din("ln2b", [1, D])
    c_ident = din("c_ident", [128, 128])
    c_uneg = din("c_uneg", [128, 128])
    c_lneg = din("c_lneg", [128, 128])
    c_u01 = din("c_u01", [128, 128])
    c_cm = din("c_cm", [128, 128])
    c_rope = din("c_rope", [L, 48])
    c_pow2 = din("c_pow2", [128, 16])
    out_d = nc.dram_tensor("out", [L, D], F32, kind="ExternalOutput").ap()
    Y_d = nc.dram_tensor("Y_scr", [L, D], BF16, kind="Internal").ap()
    X1_d = nc.dram_tensor("X1_scr", [L, D], F32, kind="Internal").ap()
    dbg = {}
    if debug:
        dbg["y"] = nc.dram_tensor("dbg_y", [L, D], F32, kind="ExternalOutput").ap()
        dbg["x1"] = nc.dram_tensor("dbg_x1", [L, D], F32, kind="ExternalOutput").ap()
        dbg["S"] = nc.dram_tensor("dbg_S", [L, L], F32, kind="ExternalOutput").ap()
        dbg["th"] = nc.dram_tensor("dbg_th", [L, 2], F32, kind="ExternalOutput").ap()

    stats = {}
    with ExitStack() as outer:
        E = outer.enter_context

        if "A" in phases:
            with ExitStack() as ph:
                EP = ph.enter_context
                P = Prog(nc, "A")

                def T(name, shape, dt):
                    return EP(nc.sbuf_tensor(name + "A", list(shape), dt)), P.buf(name)

                def OP(eng, method, reads=(), writes=(), **kw):
                    return P.op(eng, lambda e: getattr(e, method)(**kw), reads, writes)

                def MM(out, lhsT, rhs, start, stop, reads, writes, **kw):
                    return P.op("pe", lambda e: e.matmul(out, lhsT=lhsT, rhs=rhs, start=start, stop=stop, **kw), reads, writes)

                ps = PsumPool(nc, P, EP)
                identF, b_identF = T("identF", [128, 128], F32)
                identB, b_identB = T("identB", [128, 128], BF16)
                uneg, b_uneg = T("uneg", [128, 128], F32)
                lneg, b_lneg = T("lneg", [128, 128], F32)
                u01f, b_u01f = T("u01f", [128, 128], F32)
                cm, b_cm = T("cm", [128, 128], F32)
                I4, b_I4 = T("I4", [128, 512], BF16)
                pow2, b_pow2 = T("pow2", [128, 16], F32)
                ones1, b_ones1 = T("ones1", [1, 128], F32)
                b_a, b_b_a = T("b_a", [1, 256], F32)
                w_a2, b_w_a2 = T("w_a2", [16, 256], F32)
                gainB, b_gainB = T("gainB", [128, 512], F32)
                nhalf, b_nhalf = T("nhalf", [128, 8], F32)
                thneg, b_thneg = T("thneg", [128, 1], F32)
                P.dma("sp", identF[:], c_ident, writes=[b_identF], group="c", mode="all")
                P.dma("sp", uneg[:], c_uneg, writes=[b_uneg], group="c", mode="all")
                P.dma("sp", lneg[:], c_lneg, writes=[b_lneg], group="c", mode="all")
                P.dma("sp", u01f[:], c_u01, writes=[b_u01f], group="c", mode="all")
                P.dma("sp", cm[:], c_cm, writes=[b_cm], group="c", mode="all")
                P.dma("sp", pow2[:], c_pow2, writes=[b_pow2], group="c", mode="all")
                P.dma("sp", b_a[:], b_a_d, writes=[b_b_a], group="c", mode="all")
                P.dma("sp", w_a2[:], w_a2_d, writes=[b_w_a2], group="c", mode="all")
                P.dma("sp", gainB[:], AP(gain_d.tensor, 0, [[0, 128], [1, 512]]), writes=[b_gainB], group="c", mode="all")
                OP("dve", "tensor_copy", [b_identF], [b_identB], out=identB[:], in_=identF[:])
                for q in range(4):
                    OP("dve", "tensor_copy", [b_identF], [b_I4], out=I4[:, q * 128:(q + 1) * 128], in_=identF[:])
                OP("dve", "memset", [], [b_ones1], ap=ones1[:], constant=1.0)
                OP("dve", "memset", [], [b_nhalf], ap=nhalf[:], constant=-0.5)
                OP("dve", "memset", [], [b_thneg], ap=thneg[:], constant=-1.0e29)
                wA = EP(nc.sbuf_tensor("wA", [128, KC, NCOLA], BF16))
                b_wA = [P.buf(f"wA{k}") for k in range(KC)]
                for kc in range(KC):
                    P.dma("pool", wA[:, kc, :], w_in_d[kc * 128:(kc + 1) * 128, 0:NCOLA], writes=[b_wA[kc]], group=f"wA{kc}")
                kTa, b_kTa = T("kTa", [128, L], BF16)
                vaug, b_vaug = T("vaug", [128, NT, 65], BF16)
                S_r = Rot(nc, P, EP, "S_row", [128, L], F32, 2)
                jD, _u1 = T("jD", [128, 8], U8)
                jA, _u2 = T("jA", [128, 8], U8)
                state, b_state = T("state", [128, 256], F32)
                state_bf, b_state_bf = T("state_bf", [128, 256], BF16)
                OP("pool", "memset", [], [b_vaug], ap=vaug[:], constant=1.0)
                OP("pool", "memset", [], [b_state], ap=state[:], constant=0.0)
                OP("pool", "memset", [], [b_state_bf], ap=state_bf[:], constant=0.0)
                xs_r = Rot(nc, P, EP, "xs", [128, D], F32, 1)
                rp_r = Rot(nc, P, EP, "rp", [128, 48], F32, 2)
                xT_r = Rot(nc, P, EP, "xT", [128, KC, 128], BF16, 1)
                misc_r = Rot(nc, P, EP, "misc", [128, MISCW], F32, 1)
                gkiq_r = Rot(nc, P, EP, "gkiq", [128, 512], F32, 1)
                dqf_r = Rot(nc, P, EP, "dqf", [128, 512], F32, 1)
                vtok_r = Rot(nc, P, EP, "vtok", [128, 512], BF16, 1)
                rf_r = Rot(nc, P, EP, "rf", [128, 512], F32, 1)
                re_r = Rot(nc, P, EP, "re", [128, 512], F32, 1)
                alT_r = Rot(nc, P, EP, "alT", [16, 128], F32, 1)
                e1_r = Rot(nc, P, EP, "e1", [128, 256], F32, 1)
                l_r = Rot(nc, P, EP, "l", [128, 256], F32, 1)
                ebT_r = Rot(nc, P, EP, "ebT", [128, 256], F32, 1)
                enbT_r = Rot(nc, P, EP, "enbT", [128, 256], F32, 1)
                ed_r = Rot(nc, P, EP, "ed", [128, 256], F32, 1)
                qz_r = Rot(nc, P, EP, "qz", [128, 4, 128], BF16, 1)
                ktil_r = Rot(nc, P, EP, "ktil", [128, 256], BF16, 1)
                khat_r = Rot(nc, P, EP, "khat", [128, 256], BF16, 1)
                ATm_r = Rot(nc, P, EP, "ATm", [128, 512], BF16, 1)
                ssq_r = Rot(nc, P, EP, "ssq", [128, 8], F32, 2)
                sqj_r = Rot(nc, P, EP, "sqj", [128, 128], F32, 1)
                ytile_r = Rot(nc, P, EP, "ytile", [128, D], BF16, 3)
                rt_r = Rot(nc, P, EP, "rt", [128, 8, 32], F32, 1)
                q_r_r = Rot(nc, P, EP, "q_r", [128, 8, 64], BF16, 1)
                k_r_r = Rot(nc, P, EP, "k_r", [128, 128], BF16, 1)
                qi_r_r = Rot(nc, P, EP, "qi_r", [128, 8, 128], BF16, 1)
                qTa_r = Rot(nc, P, EP, "qTa", [128, 1024], BF16, 3)
                qiT2_r = Rot(nc, P, EP, "qiT2", [128, 1024], BF16, 1)
                WdT_r = Rot(nc, P, EP, "WdT", [128, 1024], BF16, 1)
                Wd_r = Rot(nc, P, EP, "Wd", [128, 8, 128], BF16, 1)
                R_r = Rot(nc, P, EP, "R", [128, 512], BF16, 4)
                NM_r = Rot(nc, P, EP, "NM", [128, 128], BF16, 4)
                PT_r = Rot(nc, P, EP, "PT", [128, 1024], BF16, 3)
                bst_r = Rot(nc, P, EP, "bst", [128, 32], F32, 2)
                bmid_r = Rot(nc, P, EP, "bmid", [128, 2], F32, 2)
                bcnt_r = Rot(nc, P, EP, "bcnt", [128, 2], F32, 2)
                bsgn_r = Rot(nc, P, EP, "bsgn", [128, 2], F32, 2)
                bu_r = Rot(nc, P, EP, "bu", [128, 2], F32, 2)
                b_junkD = P.buf("junkD")
                b_junkA = P.buf("junkA")
                rden_r = Rot(nc, P, EP, "rden", [128, 8], F32, 2)
                for (t_, b_) in qTa_r.items:
                    OP("pool", "memset", [], [b_], ap=t_[64:65, :], constant=0.0)
                for (t_, b_) in qz_r.items:
                    OP("pool", "memset", [], [b_], ap=t_[:], constant=0.0)
                for (t_, b_) in qi_r_r.items:
                    OP("pool", "memset", [], [b_], ap=t_[:], constant=0.0)
                for (t_, b_) in k_r_r.items:
                    OP("pool", "memset", [], [b_], ap=t_[:], constant=0.0)
                    OP("pool", "memset", [b_], [b_], ap=t_[:, 64:65], constant=1.0)

                tile_ctx = {}

                def A_P1(i):
                    t0 = i * 128
                    xs, b_xs = xs_r.next()
                    P.dma("sp", xs[:], x_d[t0:t0 + 128, :], writes=[b_xs], group="xs0")
                    rp, b_rp = rp_r.next()
                    P.dma("sp", rp[:], c_rope[t0:t0 + 128, :], writes=[b_rp], group=f"rp{i % 2}")
                    xT, b_xT = xT_r.next()
                    for half in range(2):
                        pb = ps.alloc()
                        for q in range(4):
                            kc = half * 4 + q
                            OP("pe", "transpose", [b_xs, b_identF], [pb[1]], out=pb[0][:, q * 128:(q + 1) * 128],
                               in_=xs[:, kc * 128:(kc + 1) * 128], identity=identF[:])
                        dst = AP(xT, half * 512, [[1024, 128], [1, 512]])
                        if half == 0:
                            OP("act", "activation", [pb[1]], [b_xT], out=dst, in_=pb[0][:], func=ACTF.Copy)
                        else:
                            OP("dve", "tensor_copy", [pb[1]], [b_xT], out=dst, in_=pb[0][:])
                        ps.free(pb)

                    def tokbank(c0, ncols):
                        pb = ps.alloc()
                        for kc in range(KC):
                            MM(pb[0][:, 0:ncols], xT[:, kc, :], wA[:, kc, c0:c0 + ncols], kc == 0, kc == KC - 1,
                               [b_xT, b_wA[kc]], [pb[1]])
                        return pb

                    yield
                    misc, b_misc = misc_r.next()
                    pb = tokbank(MISC0, MISCW)
                    OP("act", "activation", [pb[1]], [b_misc], out=misc[:], in_=pb[0][:, 0:MISCW], func=ACTF.Copy)
                    ps.free(pb)
                    yield
                    alT, b_alT = alT_r.next()
                    pb = ps.alloc()
                    MM(pb[0][0:16, 0:128], misc[:, 168:184], identF[:], True, True, [b_misc, b_identF], [pb[1]])
                    OP("act", "activation", [pb[1]], [b_alT], out=alT[:], in_=pb[0][0:16, 0:128], func=ACTF.Copy)
                    ps.free(pb)
                    pz = ps.alloc()
                    MM(pz[0][:, 0:256], ones1[0:1, :], b_a[0:1, :], True, False, [b_ones1, b_b_a], [pz[1]])
                    MM(pz[0][:, 0:256], alT[0:16, :], w_a2[0:16, :], False, True, [b_alT, b_w_a2], [pz[1]])
                    e1, b_e1 = e1_r.next()
                    lt, b_l = l_r.next()
                    OP("act", "activation", [pz[1]], [b_e1], out=e1[:], in_=pz[0][:, 0:256], func=ACTF.Exp, scale=-1.0)
                    ps.free(pz)
                    OP("act", "activation", [b_e1], [b_l], out=lt[:], in_=e1[:], func=ACTF.Ln, bias=1.0)
                    pc = ps.alloc()
                    for p in range(2):
                        MM(pc[0][:, p * 128:(p + 1) * 128], lt[:, p * 128:(p + 1) * 128], uneg[:], True, True, [b_l, b_uneg], [pc[1]])
                    MM(pc[0][:, 256:512], lneg[:], lt[:, 0:256], True, True, [b_l, b_lneg], [pc[1]])
                    ebT, b_ebT = ebT_r.next()
                    enbT, b_enbT = enbT_r.next()
                    ed, b_ed = ed_r.next()
                    OP("act", "activation", [pc[1]], [b_ebT], out=ebT[:], in_=pc[0][:, 0:256], func=ACTF.Exp)
                    OP("act", "activation", [pc[1]], [b_enbT], out=enbT[:], in_=pc[0][:, 0:256], func=ACTF.Exp, scale=-1.0)
                    OP("act", "activation", [pc[1]], [b_ed], out=ed[:], in_=pc[0][:, 256:512], func=ACTF.Exp)
                    ps.free(pc)
                    yield
                    gkiq, b_gkiq = gkiq_r.next()
                    pb = tokbank(_C["gk"], 512)
                    OP("dve", "tensor_copy", [pb[1]], [b_gkiq], out=gkiq[:], in_=pb[0][:])
                    ps.free(pb)
                    vtok, b_vtok = vtok_r.next()
                    pb = tokbank(_C["gv"], 512)
                    OP("act", "activation", [pb[1]], [b_vtok], out=vtok[:], in_=pb[0][:], func=ACTF.Copy)
                    ps.free(pb)
                    yield
                    rf, b_rf = rf_r.next()
                    re_, b_re = re_r.next()
                    pb = tokbank(_C["gr"], 512)
                    OP("dve", "tensor_copy", [pb[1]], [b_rf], out=rf[:], in_=pb[0][:])
                    OP("act", "activation", [pb[1]], [b_re], out=re_[:], in_=pb[0][:], func=ACTF.Exp, scale=-1.0)
                    ps.free(pb)
                    yield
                    OP("dve", "tensor_scalar", [b_re], [b_re], out=re_[:], in0=re_[:], scalar1=1.0, scalar2=None, op0=ALU.add)
                    OP("dve", "reciprocal", [b_re], [b_re], out=re_[:], in_=re_[:])
                    yield
                    OP("pool", "tensor_tensor", [b_rf, b_re], [b_re], out=re_[:], in0=rf[:], in1=re_[:], op=ALU.mult)
                    OP("pool", "tensor_tensor", [b_re, b_gainB], [b_re], out=re_[:], in0=re_[:], in1=gainB[:], op=ALU.mult)
                    G, b_G = re_, b_re
                    yield
                    dqf, b_dqf = dqf_r.next()
                    pb = tokbank(_C["dq"], 512)
                    OP("act", "activation", [pb[1]], [b_dqf], out=dqf[:], in_=pb[0][:], func=ACTF.Copy)
                    ps.free(pb)
                    yield
                    pf = ps.alloc()
                    for j in range(4):
                        c0 = (_C["gq"] + j * 128) if j < 2 else (_C["gk"] + (j - 2) * 128)
                        for kc in range(KC):
                            MM(pf[0][:, j * 128:(j + 1) * 128], wA[:, kc, c0:c0 + 128], xT[:, kc, :], kc == 0, kc == KC - 1,
                               [b_xT, b_wA[kc]], [pf[1]])
                    qz, b_qz = qz_r.next()
                    ktil, b_ktil = ktil_r.next()
                    khat, b_khat = khat_r.next()
                    for h in range(4):
                        p, hh = divmod(h, 2)
                        OP("dve", "scalar_tensor_tensor", [pf[1], b_ebT], [b_qz], out=qz[hh * 64:(hh + 1) * 64, h, :],
                           in0=pf[0][hh * 64:(hh + 1) * 64, p * 128:(p + 1) * 128], scalar=0.125,
                           in1=ebT[hh * 64:(hh + 1) * 64, p * 128:(p + 1) * 128], op0=ALU.mult, op1=ALU.mult)
                    OP("dve", "tensor_tensor", [pf[1], b_enbT], [b_ktil], out=ktil[:], in0=pf[0][:, 256:512], in1=enbT[:], op=ALU.mult)
                    ps.free(pf)
                    OP("pool", "tensor_tensor", [b_gkiq, b_ed], [b_khat], out=khat[:], in0=gkiq[:, 0:256], in1=ed[:], op=ALU.mult)
                    yield
                    pa = ps.alloc()
                    for h in range(4):
                        p, hh = divmod(h, 2)
                        MM(pa[0][:, h * 128:(h + 1) * 128], ktil[:, p * 128:(p + 1) * 128],
                           qz[:, h, :], True, True, [b_ktil, b_qz], [pa[1]])
                    ATm, b_ATm = ATm_r.next()
                    OP("dve", "tensor_tensor", [pa[1], b_u01f], [b_ATm], out=AP(ATm, 0, [[512, 128], [128, 4], [1, 128]]),
                       in0=AP(pa[0], 0, [[512, 128], [128, 4], [1, 128]]), in1=AP(u01f, 0, [[128, 128], [0, 4], [1, 128]]), op=ALU.mult)
                    ps.free(pa)
                    po = ps.alloc()
                    for h in range(4):
                        p, hh = divmod(h, 2)
                        MM(po[0][:, h * 128:(h + 1) * 128], qz[:, h, :],
                           state_bf[:, p * 128:(p + 1) * 128], True, False, [b_qz, b_state_bf], [po[1]])
                        MM(po[0][:, h * 128:(h + 1) * 128], ATm[:, h * 128:(h + 1) * 128], vtok[:, h * 128:(h + 1) * 128],
                           False, True, [b_ATm, b_vtok], [po[1]])
                    yield
                    pu = ps.alloc()
                    for p in range(2):
                        MM(pu[0][:, p * 256:(p + 1) * 256], khat[:, p * 128:(p + 1) * 128], vtok[:, p * 256:(p + 1) * 256], True, True,
                           [b_khat, b_vtok], [pu[1]])
                    for p in range(2):
                        for hh in range(2):
                            OP("dve", "scalar_tensor_tensor", [b_state, b_ebT, pu[1]], [b_state],
                               out=state[hh * 64:(hh + 1) * 64, p * 128:(p + 1) * 128],
                               in0=state[hh * 64:(hh + 1) * 64, p * 128:(p + 1) * 128],
                               scalar=ebT[hh * 64:(hh + 1) * 64, p * 128 + 127:p * 128 + 128],
                               in1=pu[0][hh * 64:(hh + 1) * 64, p * 256 + hh * 128:p * 256 + hh * 128 + 128],
                               op0=ALU.mult, op1=ALU.add)
                    ps.free(pu)
                    OP("pool", "tensor_copy", [b_state], [b_state_bf], out=state_bf[:], in_=state[:])
                    yield
                    ssq, b_ssq = ssq_r.next()
                    sqj, b_sqj = sqj_r.next()
                    for h in range(4):
                        OP("act", "activation", [po[1]], [b_sqj, b_ssq], out=sqj[:], in_=po[0][:, h * 128:(h + 1) * 128], func=ACTF.Square,
                           accum_out=ssq[:, h:h + 1])
                    OP("dve", "tensor_scalar", [b_ssq], [b_ssq], out=ssq[:, 4:8], in0=ssq[:, 0:4], scalar1=1.0 / 128.0, scalar2=RMS_EPS,
                       op0=ALU.mult, op1=ALU.add)
                    OP("pool", "tensor_tensor", [b_ssq, b_nhalf], [b_ssq], out=ssq[:, 0:4], in0=ssq[:, 4:8], in1=nhalf[:, 0:4], op=ALU.pow)
                    ytile, b_ytile = ytile_r.next()
                    for h in range(4):
                        OP("dve", "scalar_tensor_tensor", [po[1], b_ssq, b_G], [b_ytile], out=ytile[:, h * 128:(h + 1) * 128],
                           in0=po[0][:, h * 128:(h + 1) * 128], scalar=ssq[:, h:h + 1], in1=G[:, h * 128:(h + 1) * 128],
                           op0=ALU.mult, op1=ALU.mult)
                    ps.free(po)

                    yield
                    rt, b_rt = rt_r.next()
                    q_r, b_q_r = q_r_r.next()
                    k_r, b_k_r = k_r_r.next()
                    qi_r, b_qi_r = qi_r_r.next()

                    def rope(src_t, src_off, src_pstride, nh, hd, half, tab0, dst_t, dst_off, dst_pstride, rd, wr, dhd=None):
                        dhd = hd if dhd is None else dhd
                        rot = 2 * half
                        sA = AP(src_t, src_off, [[src_pstride, 128], [hd, nh], [1, rot]])
                        cc = AP(rp, tab0, [[48, 128], [0, nh], [1, rot]])
                        tA = AP(rt, 0, [[256, 128], [16, nh], [1, rot]])
                        tB = AP(rt, 128, [[256, 128], [16, nh], [1, rot]])
                        OP("pool", "tensor_tensor", rd + [b_rp], [b_rt], out=tA, in0=sA, in1=cc, op=ALU.mult)
                        s2 = AP(src_t, src_off + half, [[src_pstride, 128], [hd, nh], [1, half]])
                        s1 = AP(src_t, src_off, [[src_pstride, 128], [hd, nh], [1, half]])
                        nsin = AP(rp, tab0 + rot, [[48, 128], [0, nh], [1, half]])
                        psin = AP(rp, tab0 + rot + half, [[48, 128], [0, nh], [1, half]])
                        tB1 = AP(rt, 128, [[256, 128], [16, nh], [1, half]])
                        tB2 = AP(rt, 128 + half, [[256, 128], [16, nh], [1, half]])
                        OP("pool", "tensor_tensor", rd + [b_rp], [b_rt], out=tB1, in0=s2, in1=nsin, op=ALU.mult)
                        OP("pool", "tensor_tensor", rd + [b_rp], [b_rt], out=tB2, in0=s1, in1=psin, op=ALU.mult)
                        dR = AP(dst_t, dst_off, [[dst_pstride, 128], [dhd, nh], [1, rot]])
                        OP("pool", "tensor_tensor", [b_rt], wr, out=dR, in0=tA, in1=tB, op=ALU.add)
                        sN = AP(src_t, src_off + rot, [[src_pstride, 128], [hd, nh], [1, hd - rot]])
                        dN = AP(dst_t, dst_off + rot, [[dst_pstride, 128], [dhd, nh], [1, hd - rot]])
                        OP("pool", "tensor_copy", rd, wr, out=dN, in_=sN)

                    rope(dqf, 0, 512, 8, 64, 8, 0, q_r, 0, 512, [b_dqf], [b_q_r])
                    rope(misc, 0, MISCW, 1, 64, 8, 0, k_r, 0, 128, [b_misc], [b_k_r])
                    rope(gkiq, 256, 512, 8, 32, 4, 32, qi_r, 96, 1024, [b_gkiq], [b_qi_r], dhd=128)
                    rope(misc, 128, MISCW, 1, 32, 4, 32, k_r, 96, 128, [b_misc], [b_k_r])
                    OP("pool", "tensor_copy", [b_misc], [b_vaug], out=vaug[:, i, 0:64], in_=misc[:, 64:128])
                    yield
                    qTa, b_qTa = qTa_r.next()
                    for b2 in range(2):
                        pq = ps.alloc()
                        for hh in range(4):
                            h = b2 * 4 + hh
                            MM(pq[0][0:64, hh * 128:(hh + 1) * 128], q_r[:, h, :], identB[:], True, True, [b_q_r, b_identB], [pq[1]])
                        OP("act", "activation", [pq[1]], [b_qTa], out=qTa[0:64, b2 * 512:(b2 + 1) * 512], in_=pq[0][0:64, :],
                           func=ACTF.Copy, scale=0.125)
                        ps.free(pq)
                    pk = ps.alloc()
                    MM(pk[0][:, 0:128], k_r[:, :], identB[:], True, True, [b_k_r, b_identB], [pk[1]])
                    OP("dve", "tensor_copy", [pk[1]], [b_kTa], out=kTa[:, t0:t0 + 128], in_=pk[0][:, 0:128])
                    ps.free(pk)
                    qiT2, b_qiT2 = qiT2_r.next()
                    for b2 in range(2):
                        pi_ = ps.alloc()
                        for hh in range(4):
                            h = b2 * 4 + hh
                            MM(pi_[0][:, hh * 128:(hh + 1) * 128], qi_r[:, h, :], identB[:], True, True, [b_qi_r, b_identB], [pi_[1]])
                        src = AP(pi_[0], 0, [[512, 128], [16, 8], [128, 4], [1, 16]])
                        dst = AP(qiT2, b2 * 64, [[1024, 128], [128, 8], [16, 4], [1, 16]])
                        OP("act", "activation", [pi_[1]], [b_qiT2], out=dst, in_=src, func=ACTF.Copy)
                        ps.free(pi_)
                    WdT, b_WdT = WdT_r.next()
                    in0 = AP(misc, 160, [[MISCW, 128], [0, 8], [1, 8], [0, 16]])
                    in1 = AP(identF, 0, [[128, 128], [16, 8], [0, 8], [1, 16]])
                    outw = AP(WdT, 0, [[1024, 128], [128, 8], [16, 8], [1, 16]])
                    OP("dve", "tensor_tensor", [b_misc, b_identF], [b_WdT], out=outw, in0=in0, in1=in1, op=ALU.mult)
                    Wd, b_Wd = Wd_r.next()
                    for b2 in range(2):
                        pw = ps.alloc()
                        for gg in range(4):
                            g = b2 * 4 + gg
                            MM(pw[0][:, gg * 128:(gg + 1) * 128], WdT[:, g * 128:(g + 1) * 128], identB[:], True, True, [b_WdT, b_identB], [pw[1]])
                        dst = AP(Wd, b2 * 512, [[1024, 128], [1, 512]])
                        if b2 == 0:
                            OP("dve", "tensor_copy", [pw[1]], [b_Wd], out=dst, in_=pw[0][:])
                        else:
                            OP("act", "activation", [pw[1]], [b_Wd], out=dst, in_=pw[0][:], func=ACTF.Copy)
                        ps.free(pw)
                    tile_ctx[i] = dict(qTa=(qTa, b_qTa), qiT2=(qiT2, b_qiT2), Wd=(Wd, b_Wd), ytile=(ytile, b_ytile))

                def A_P2(i):
                    t0 = i * 128
                    n_i = t0 + 128
                    qiT2, b_qiT2 = tile_ctx[i]["qiT2"]
                    Wd, b_Wd = tile_ctx[i]["Wd"]
                    S_row, b_S = S_r.next()
                    tile_ctx[i]["S"] = (S_row, b_S)
                    nch = (n_i + 511) // 512
                    LA = 2
                    pend = []
                    pS_cur = {}

                    def score(item):
                        c5, g, w5, R, b_R = item
                        if g == 0:
                            pS_cur[c5] = ps.alloc()
                        pS = pS_cur[c5]
                        MM(pS[0][:, 0:w5], Wd[:, g, :], R[:, 0:w5], g == 0, g == 7, [b_Wd, b_R], [pS[1]])
                        if g == 7:
                            last = (c5 == nch - 1)
                            wplain = w5 - 128 if last else w5
                            if wplain > 0:
                                OP("act", "activation", [pS[1]], [b_S], out=S_row[:, c5 * 512:c5 * 512 + wplain], in_=pS[0][:, 0:wplain],
                                   func=ACTF.Copy)
                            if last:
                                OP("dve", "tensor_tensor", [pS[1], b_cm], [b_S], out=S_row[:, t0:t0 + 128], in0=pS[0][:, w5 - 128:w5],
                                   in1=cm[:], op=ALU.add)
                            ps.free(pS)

                    k = 0
                    for c5 in range(nch):
                        w5 = min(512, n_i - c5 * 512)
                        for g in range(8):
                            pL = ps.alloc()
                            MM(pL[0][:, 0:w5], qiT2[:, g * 128:(g + 1) * 128], kTa[:, c5 * 512:c5 * 512 + w5], True, True,
                               [b_qiT2, b_kTa], [pL[1]])
                            R, b_R = R_r.next()
                            if k % 2 == 0:
                                OP("act", "activation", [pL[1]], [b_R], out=R[:, 0:w5], in_=pL[0][:, 0:w5], func=ACTF.Relu)
                            else:
                                OP("dve", "tensor_scalar", [pL[1]], [b_R], out=R[:, 0:w5], in0=pL[0][:, 0:w5], scalar1=0.0, scalar2=None,
                                   op0=ALU.max)
                            k += 1
                            ps.free(pL)
                            pend.append((c5, g, w5, R, b_R))
                            if len(pend) > LA:
                                score(pend.pop(0))
                            yield
                    while pend:
                        score(pend.pop(0))
                    if debug:
                        P.dma("sp", dbg["S"][t0:t0 + 128, 0:n_i], S_row[:, 0:n_i], reads=[b_S], group="dbgS")

                def A_P3(i):
                    t0 = i * 128
                    n_i = t0 + 128
                    S_row, b_S = tile_ctx[i]["S"]
                    if n_i <= topk:
                        tile_ctx[i]["theta"] = (thneg[:, 0:1], b_thneg)
                        return
                    a = int(round(0.45 * n_i / 128.0)) * 128
                    a = max(128, min(a, n_i - 128))
                    n_act = n_i - a
                    bst, b_st = bst_r.next()
                    bmid, b_mid = bmid_r.next()
                    bcnt, b_cnt = bcnt_r.next()
                    bsgn, b_sgn = bsgn_r.next()
                    bu, b_u = bu_r.next()
                    OP("dve", "tensor_reduce", [b_S], [b_st], out=bst[:, 0:1], in_=S_row[:, 0:n_i], axis=AX.X, op=ALU.max)
                    OP("dve", "tensor_reduce", [b_S, b_st], [b_st], out=bst[:, 1:2], in_=S_row[:, 0:t0], axis=AX.X, op=ALU.min)
                    OP("dve", "tensor_tensor", [b_st], [b_st], out=bst[:, 2:3], in0=bst[:, 0:1], in1=bst[:, 1:2], op=ALU.subtract)
                    OP("dve", "tensor_scalar", [b_st, b_pow2], [b_st], out=bst[:, 8:8 + nbis + 1], in0=pow2[:, 0:nbis + 1], scalar1=bst[:, 2:3],
                       scalar2=None, op0=ALU.mult)
                    OP("dve", "tensor_copy", [b_st], [b_st], out=bst[:, 8 + nbis:9 + nbis], in_=bst[:, 7 + nbis:8 + nbis])
                    OP("dve", "tensor_tensor", [b_st], [b_mid], out=bmid[:, 0:1], in0=bst[:, 1:2], in1=bst[:, 8:9], op=ALU.add)
                    for k in range(nbis):
                        dst = 0 if k < nbis - 1 else 1
                        OP("dve", "tensor_scalar", [b_S, b_mid], [b_junkD, b_cnt], out=AP(jD, 0, [[8, 128], [0, a]]), in0=S_row[:, 0:a], scalar1=bmid[:, 0:1],
                           scalar2=None, op0=ALU.is_ge, op1=ALU.add, accum_out=bcnt[:, 0:1])
                        OP("act", "activation", [b_S, b_mid], [b_junkA, b_sgn], out=AP(jA, 0, [[8, 128], [0, n_act]]), in_=S_row[:, a:n_i], func=ACTF.Sign,
                           scale=-1.0, bias=bmid[:, 0:1], accum_out=bsgn[:, 0:1])
                        OP("dve", "scalar_tensor_tensor", [b_cnt, b_sgn], [b_u], out=bu[:, 0:1], in0=bcnt[:, 0:1], scalar=2.0, in1=bsgn[:, 0:1],
                           op0=ALU.mult, op1=ALU.subtract)
                        OP("dve", "tensor_scalar", [b_u, b_st], [b_u], out=bu[:, 1:2], in0=bu[:, 0:1], scalar1=float(2 * topk - n_act),
                           scalar2=bst[:, 8 + k:9 + k], op0=ALU.is_ge, op1=ALU.mult)
                        OP("dve", "scalar_tensor_tensor", [b_u, b_st, b_mid], [b_mid], out=bmid[:, dst:dst + 1], in0=bu[:, 1:2],
                           scalar=bst[:, 9 + k:10 + k], in1=bmid[:, 0:1], op0=ALU.subtract, op1=ALU.add)
                        yield
                    tile_ctx[i]["theta"] = (bmid[:, 1:2], b_mid)

                def A_P4(i):
                    t0 = i * 128
                    qTa, b_qTa = tile_ctx[i]["qTa"]
                    ytile, b_ytile = tile_ctx[i]["ytile"]
                    theta, b_theta = tile_ctx[i]["theta"]
                    S_row, b_S = tile_ctx[i]["S"]
                    pO = [ps.alloc(), ps.alloc()]

                    def pv(c, PT, b_PT):
                        for h in range(8):
                            b2, hh = divmod(h, 4)
                            MM(pO[b2][0][:, hh * 65:(hh + 1) * 65], PT[:, h * 128:(h + 1) * 128], vaug[:, c, :], (c == 0 and hh == 0), (c == i),
                               [b_PT, b_vaug], [pO[b2][1]], skip_group_check=True)

                    prev = None
                    for c in range(i + 1):
                        NM, b_NM = NM_r.next()
                        OP("dve", "tensor_scalar", [b_S, b_theta], [b_NM], out=NM[:], in0=S_row[:, c * 128:(c + 1) * 128], scalar1=theta,
                           scalar2=MASK_NEG, op0=ALU.is_lt, op1=ALU.mult)
                        PT, b_PT = PT_r.next()
                        for hf in range(2):
                            pL = ps.alloc()
                            MM(pL[0][:], kTa[0:65, c * 128:(c + 1) * 128], qTa[0:65, hf * 512:(hf + 1) * 512], True, False,
                               [b_kTa, b_qTa], [pL[1]])
                            MM(pL[0][:], NM[:], I4[:], False, True, [b_NM, b_I4], [pL[1]])
                            OP("act", "activation", [pL[1]], [b_PT], out=PT[:, hf * 512:(hf + 1) * 512], in_=pL[0][:], func=ACTF.Exp)
                            ps.free(pL)
                        if prev is not None:
                            pv(*prev)
                        prev = (c, PT, b_PT)
                        yield
                    pv(*prev)
                    rden, b_rden = rden_r.next()
                    for b2 in range(2):
                        den = AP(pO[b2][0], 64, [[512, 128], [65, 4]])
                        OP("dve", "reciprocal", [pO[b2][1]], [b_rden], out=rden[:, b2 * 4:(b2 + 1) * 4], in_=den)
                        num = AP(pO[b2][0], 0, [[512, 128], [65, 4], [1, 64]])
                        rb = AP(rden, b2 * 4, [[8, 128], [1, 4], [0, 64]])
                        dsty = AP(ytile, 512 + b2 * 256, [[1024, 128], [64, 4], [1, 64]])
                        OP("dve", "tensor_tensor", [pO[b2][1], b_rden], [b_ytile], out=dsty, in0=num, in1=rb, op=ALU.mult)
                        ps.free(pO[b2])
                    P.dma("pool", Y_d[t0:t0 + 128, :], ytile[:], reads=[b_ytile], group=f"yst{i % 3}")
                    if debug:
                        yf, b_yf = xs_r.next()
                        OP("pool", "tensor_copy", [b_ytile], [b_yf], out=yf[:], in_=ytile[:])
                        P.dma("sp", dbg["y"][t0:t0 + 128, :], yf[:], reads=[b_yf], group="dbgy")

                def drive(ga, gb=None, ratio=1):
                    a_live, b_live = True, gb is not None
                    while a_live or b_live:
                        if b_live:
                            try:
                                next(gb)
                            except StopIteration:
                                b_live = False
                        for _ in range(ratio):
                            if a_live:
                                try:
                                    next(ga)
                                except StopIteration:
                                    a_live = False

                drive(A_P1(0))
                drive(A_P2(0))
                if NT > 1:
                    drive(A_P1(1))
                import os as _os
                ov_a = _os.environ.get("OVA", "1") == "1"
                ov_b = _os.environ.get("OVB", "1") == "1"
                for i in range(NT):
                    if i + 1 < NT:
                        n_items = 8 * ((128 * (i + 2) + 511) // 512)
                        if ov_a:
                            drive(A_P2(i + 1), A_P3(i), ratio=max(1, -(-n_items // (nbis + 1))))
                        else:
                            drive(A_P3(i))
                            drive(A_P2(i + 1))
                    else:
                        drive(A_P3(i))
                    if i + 2 < NT:
                        if ov_b:
                            drive(A_P4(i), A_P1(i + 2), ratio=max(1, -(-(i + 1) // 13)))
                        else:
                            drive(A_P4(i))
                            drive(A_P1(i + 2))
                    else:
                        drive(A_P4(i))
                P.emit(E)
                P.barrier()
                stats["A"] = (len(P.all_ops), P.n_wait)

        if "B" in phases:
            with ExitStack() as ph:
                EP = ph.enter_context
                P = Prog(nc, "B")

                def T(name, shape, dt):
                    return EP(nc.sbuf_tensor(name + "B", list(shape), dt)), P.buf(name)

                def OP(eng, method, reads=(), writes=(), **kw):
                    return P.op(eng, lambda e: getattr(e, method)(**kw), reads, writes)

                def MM(out, lhsT, rhs, start, stop, reads, writes, **kw):
                    return P.op("pe", lambda e: e.matmul(out, lhsT=lhsT, rhs=rhs, start=start, stop=stop, **kw), reads, writes)

                ps = PsumPool(nc, P, EP)
                identF, b_identF = T("identF", [128, 128], F32)
                identB, b_identB = T("identB", [128, 128], BF16)
                g1B, b_g1B = T("g1B", [128, D], F32)
                b1B, b_b1B = T("b1B", [128, D], F32)
                nhalf, b_nhalf = T("nhalf", [128, 2], F32)
                P.dma("sp", identF[:], c_ident, writes=[b_identF], group="c", mode="all")
                P.dma("sp", g1B[:], AP(ln1g_d.tensor, 0, [[0, 128], [1, D]]), writes=[b_g1B], group="c", mode="all")
                P.dma("sp", b1B[:], AP(ln1b_d.tensor, 0, [[0, 128], [1, D]]), writes=[b_b1B], group="c", mode="all")
                OP("dve", "tensor_copy", [b_identF], [b_identB], out=identB[:], in_=identF[:])
                OP("dve", "memset", [], [b_nhalf], ap=nhalf[:], constant=-0.5)
                wG = EP(nc.sbuf_tensor("wG", [128, KC, 2048], BF16))
                wBr = EP(nc.sbuf_tensor("wBr", [128, KC, D], BF16))
                wO = EP(nc.sbuf_tensor("wO", [128, KC, D], BF16))
                b_wG = [P.buf(f"wG{k}") for k in range(KC)]
                b_wBr = [P.buf(f"wBr{k}") for k in range(KC)]
                b_wO = [P.buf(f"wO{k}") for k in range(KC)]
                for kc in range(KC):
                    P.dma("pool", wG[:, kc, :], w_in_d[kc * 128:(kc + 1) * 128, NCOLA:NCOLA + 2048], writes=[b_wG[kc]], group=f"wG{kc}")
                    P.dma("pool", wBr[:, kc, :], w_br_d[kc * 128:(kc + 1) * 128, :], writes=[b_wBr[kc]], group=f"wBr{kc}")
                    P.dma("pool", wO[:, kc, :], w_o_d[kc * 128:(kc + 1) * 128, :], writes=[b_wO[kc]], group=f"wO{kc}")
                xs_r = Rot(nc, P, EP, "xs", [128, D], F32, 3)
                ys_r = Rot(nc, P, EP, "ys", [128, D], BF16, 3)
                xT_r = Rot(nc, P, EP, "xT", [128, KC, 128], BF16, 2)
                yT_r = Rot(nc, P, EP, "yT", [128, KC, 128], BF16, 2)
                sg_r = Rot(nc, P, EP, "sg", [128, 512], F32, 3)
                m0_r = Rot(nc, P, EP, "m0", [128, 512], F32, 3)
                m1_r = Rot(nc, P, EP, "m1", [128, 512], F32, 3)
                mT_r = Rot(nc, P, EP, "mT", [128, KC, 128], BF16, 2)
                yres_r = Rot(nc, P, EP, "yres", [128, D], F32, 2)
                x1_r = Rot(nc, P, EP, "x1", [128, D], F32, 2)
                st_r = Rot(nc, P, EP, "st", [128, 16], F32, 2)

                def layer_norm(src, b_src, dst, b_dst, gB, b_gB, bB, b_bB, st, b_st, nh, b_nh):
                    for q in range(2):
                        OP("dve", "bn_stats", [b_src], [b_st], out=st[:, q * 6:(q + 1) * 6], in_=src[:, q * 512:(q + 1) * 512])
                    OP("dve", "bn_aggr", [b_st], [b_st], out=st[:, 12:14], in_=st[:, 0:12])
                    OP("dve", "tensor_scalar", [b_st], [b_st], out=st[:, 14:15], in0=st[:, 13:14], scalar1=LN_EPS, scalar2=None, op0=ALU.add)
                    OP("pool", "tensor_tensor", [b_st, b_nh], [b_st], out=st[:, 15:16], in0=st[:, 14:15], in1=nh[:, 0:1], op=ALU.pow)
                    OP("dve", "tensor_scalar", [b_src, b_st], [b_dst], out=dst[:], in0=src[:], scalar1=st[:, 12:13], scalar2=st[:, 15:16],
                       op0=ALU.subtract, op1=ALU.mult)
                    OP("pool", "tensor_tensor", [b_dst, b_gB], [b_dst], out=dst[:], in0=dst[:], in1=gB[:], op=ALU.mult)
                    OP("pool", "tensor_tensor", [b_dst, b_bB], [b_dst], out=dst[:], in0=dst[:], in1=bB[:], op=ALU.add)

                def B_tile(i):
                    t0 = i * 128
                    xs, b_xs = xs_r.next()
                    ys, b_ys = ys_r.next()
                    P.dma("sp", xs[:], x_d[t0:t0 + 128, :], writes=[b_xs], group=f"xs{i % 3}")
                    P.dma("sp", ys[:], Y_d[t0:t0 + 128, :], writes=[b_ys], group=f"ys{i % 3}")
                    xT, b_xT = xT_r.next()
                    yT, b_yT = yT_r.next()
                    for half in range(2):
                        pb = ps.alloc()
                        for q in range(4):
                            kc = half * 4 + q
                            OP("pe", "transpose", [b_xs, b_identF], [pb[1]], out=pb[0][:, q * 128:(q + 1) * 128],
                               in_=xs[:, kc * 128:(kc + 1) * 128], identity=identF[:])
                        OP("act", "activation", [pb[1]], [b_xT], out=AP(xT, half * 512, [[1024, 128], [1, 512]]), in_=pb[0][:], func=ACTF.Copy)
                        ps.free(pb)
                        pb = ps.alloc()
                        for q in range(4):
                            kc = half * 4 + q
                            MM(pb[0][:, q * 128:(q + 1) * 128], ys[:, kc * 128:(kc + 1) * 128], identB[:], True, True, [b_ys, b_identB], [pb[1]])
                        OP("dve", "tensor_copy", [pb[1]], [b_yT], out=AP(yT, half * 512, [[1024, 128], [1, 512]]), in_=pb[0][:])
                        ps.free(pb)
                    mT, b_mT = mT_r.next()
                    for half in range(2):
                        held = None
                        for n in range(2):
                            pg = ps.alloc()
                            pp = ps.alloc()
                            for q in range(4):
                                dc = half * 4 + q
                                c0 = n * 1024 + dc * 128
                                for kc in range(KC):
                                    MM(pg[0][:, q * 128:(q + 1) * 128], wG[:, kc, c0:c0 + 128], xT[:, kc, :], kc == 0, kc == KC - 1,
                                       [b_wG[kc], b_xT], [pg[1]])
                                for cc in range(4):
                                    MM(pp[0][:, q * 128:(q + 1) * 128], wBr[:, n * 4 + cc, dc * 128:(dc + 1) * 128], yT[:, n * 4 + cc, :], cc == 0, cc == 3,
                                       [b_wBr[n * 4 + cc], b_yT], [pp[1]])
                            sg, b_sg = sg_r.next()
                            OP("act", "activation", [pg[1]], [b_sg], out=sg[:], in_=pg[0][:], func=ACTF.Sigmoid)
                            ps.free(pg)
                            if n == 0:
                                m0, b_m0 = m0_r.next()
                                OP("dve", "tensor_tensor", [pp[1], b_sg], [b_m0], out=m0[:], in0=pp[0][:], in1=sg[:], op=ALU.mult)
                                held = (m0, b_m0)
                            else:
                                m1, b_m1 = m1_r.next()
                                OP("dve", "tensor_tensor", [pp[1], b_sg], [b_m1], out=m1[:], in0=pp[0][:], in1=sg[:], op=ALU.mult)
                                OP("pool", "tensor_tensor", [b_m1, held[1]], [b_mT], out=AP(mT, half * 512, [[1024, 128], [1, 512]]), in0=m1[:], in1=held[0][:],
                                   op=ALU.add)
                            ps.free(pp)
                    yres, b_yres = yres_r.next()
                    for nb in range(2):
                        pm = ps.alloc()
                        for dc in range(KC):
                            MM(pm[0][:], mT[:, dc, :], wO[:, dc, nb * 512:(nb + 1) * 512], dc == 0, dc == KC - 1, [b_mT, b_wO[dc]], [pm[1]])
                        OP("dve", "scalar_tensor_tensor", [b_xs, pm[1]], [b_yres], out=yres[:, nb * 512:(nb + 1) * 512], in0=xs[:, nb * 512:(nb + 1) * 512],
                           scalar=ALPHA, in1=pm[0][:], op0=ALU.mult, op1=ALU.add)
                        ps.free(pm)
                    x1, b_x1 = x1_r.next()
                    st, b_st = st_r.next()
                    layer_norm(yres, b_yres, x1, b_x1, g1B, b_g1B, b1B, b_b1B, st, b_st, nhalf, b_nhalf)
                    P.dma("pool", X1_d[t0:t0 + 128, :], x1[:], reads=[b_x1], group=f"x1st{i % 2}")
                    if debug:
                        P.dma("sp", dbg["x1"][t0:t0 + 128, :], x1[:], reads=[b_x1], group="dbgx1")

                for i in range(NT if "B" in phases else 0):
                    B_tile(i)
                P.emit(E)
                P.barrier()
                stats["B"] = (len(P.all_ops), P.n_wait)

        if "C" in phases:
            with ExitStack() as ph:
                EP = ph.enter_context
                P = Prog(nc, "C")
                ST = 256 if L % 256 == 0 else 128
                NS = L // ST
                TPS = ST // 128

                def T(name, shape, dt):
                    return EP(nc.sbuf_tensor(name + "C", list(shape), dt)), P.buf(name)

                def OP(eng, method, reads=(), writes=(), **kw):
                    return P.op(eng, lambda e: getattr(e, method)(**kw), reads, writes)

                def MM(out, lhsT, rhs, start, stop, reads, writes, **kw):
                    return P.op("pe", lambda e: e.matmul(out, lhsT=lhsT, rhs=rhs, start=start, stop=stop, **kw), reads, writes)

                ps = PsumPool(nc, P, EP)
                identF, b_identF = T("identF", [128, 128], F32)
                g2B, b_g2B = T("g2B", [128, D], F32)
                b2B, b_b2B = T("b2B", [128, D], F32)
                nhalf, b_nhalf = T("nhalf", [128, 2], F32)
                cw, b_cw = T("cw", [128, 3, 2 * NFC], F32)
                cb, b_cb = T("cb", [128, 2 * NFC], F32)
                halo, b_halo = T("halo", [128, 2 * NFC, 2], F32)
                P.dma("sp", identF[:], c_ident, writes=[b_identF], group="c", mode="all")
                P.dma("sp", g2B[:], AP(ln2g_d.tensor, 0, [[0, 128], [1, D]]), writes=[b_g2B], group="c", mode="all")
                P.dma("sp", b2B[:], AP(ln2b_d.tensor, 0, [[0, 128], [1, D]]), writes=[b_b2B], group="c", mode="all")
                cwraw, _unused = T("cwraw", [2 * NFC, 4, 128], F32)
                b_cwr = [P.buf(f"cwraw{j}") for j in range(4)]
                for j in range(3):
                    P.dma("sp", cwraw[:, j, :], AP(cw_d.tensor, j * 2 * D_FF, [[128, 2 * NFC], [1, 128]]), writes=[b_cwr[j]], group="c", mode="all")
                P.dma("sp", cwraw[:, 3, :], AP(cb_d.tensor, 0, [[128, 2 * NFC], [1, 128]]), writes=[b_cwr[3]], group="c", mode="all")
                pcw = ps.alloc()
                for j in range(4):
                    OP("pe", "transpose", [b_cwr[j], b_identF], [pcw[1]], out=pcw[0][:, j * 2 * NFC:(j + 1) * 2 * NFC], in_=cwraw[:, j, :],
                       identity=identF[0:2 * NFC, 0:2 * NFC])
                OP("act", "activation", [pcw[1]], [b_cw], out=AP(cw, 0, [[6 * NFC, 128], [1, 6 * NFC]]), in_=pcw[0][:, 0:6 * NFC], func=ACTF.Copy)
                OP("act", "activation", [pcw[1]], [b_cb], out=cb[:], in_=pcw[0][:, 6 * NFC:8 * NFC], func=ACTF.Copy)
                ps.free(pcw)
                OP("dve", "memset", [], [b_nhalf], ap=nhalf[:], constant=-0.5)
                OP("dve", "memset", [], [b_halo], ap=halo[:], constant=0.0)
                wU = EP(nc.sbuf_tensor("wU", [128, KC, 2 * D_FF], BF16))
                wDn = EP(nc.sbuf_tensor("wDn", [128, NFC, D], BF16))
                b_wU = [P.buf(f"wU{k}") for k in range(KC)]
                b_wDn = [P.buf(f"wDn{k}") for k in range(NFC)]
                for kc in range(KC):
                    P.dma("pool", wU[:, kc, :], w_up_d[kc * 128:(kc + 1) * 128, :], writes=[b_wU[kc]], group=f"wU{kc}")
                for j in range(NFC):
                    P.dma("pool", wDn[:, j, :], w_dn_d[j * 128:(j + 1) * 128, :], writes=[b_wDn[j]], group=f"wD{j}")
                xs_r = Rot(nc, P, EP, "xs", [128, D], F32, 2 * TPS)
                xT_r = Rot(nc, P, EP, "xT", [128, KC, ST], BF16, 2)
                upb_r = Rot(nc, P, EP, "upb", [128, ST + 2], F32, 4)
                u_r = Rot(nc, P, EP, "u", [128, ST], F32, 4)
                sl_r = Rot(nc, P, EP, "sl", [128, ST], F32, 2)
                gT_r = Rot(nc, P, EP, "gT", [128, NFC, ST], BF16, 1)
                yres_r = Rot(nc, P, EP, "yres", [128, D], F32, 1)
                o_r = Rot(nc, P, EP, "o", [128, D], F32, 2)
                st_r = Rot(nc, P, EP, "st", [128, 16], F32, 2)

                def layer_norm(src, b_src, dst, b_dst, gB, b_gB, bB, b_bB, st, b_st, nh, b_nh):
                    for q in range(2):
                        OP("dve", "bn_stats", [b_src], [b_st], out=st[:, q * 6:(q + 1) * 6], in_=src[:, q * 512:(q + 1) * 512])
                    OP("dve", "bn_aggr", [b_st], [b_st], out=st[:, 12:14], in_=st[:, 0:12])
                    OP("dve", "tensor_scalar", [b_st], [b_st], out=st[:, 14:15], in0=st[:, 13:14], scalar1=LN_EPS, scalar2=None, op0=ALU.add)
                    OP("pool", "tensor_tensor", [b_st, b_nh], [b_st], out=st[:, 15:16], in0=st[:, 14:15], in1=nh[:, 0:1], op=ALU.pow)
                    OP("dve", "tensor_scalar", [b_src, b_st], [b_dst], out=dst[:], in0=src[:], scalar1=st[:, 12:13], scalar2=st[:, 15:16],
                       op0=ALU.subtract, op1=ALU.mult)
                    OP("pool", "tensor_tensor", [b_dst, b_gB], [b_dst], out=dst[:], in0=dst[:], in1=gB[:], op=ALU.mult)
                    OP("pool", "tensor_tensor", [b_dst, b_bB], [b_dst], out=dst[:], in0=dst[:], in1=bB[:], op=ALU.add)

                def conv_chunk(ch, xT, b_xT):
                    pu = ps.alloc()
                    for kc in range(KC):
                        MM(pu[0][:, 0:ST], wU[:, kc, ch * 128:(ch + 1) * 128], xT[:, kc, :], kc == 0, kc == KC - 1, [b_wU[kc], b_xT], [pu[1]])
                    upb, b_upb = upb_r.next()
                    OP("pool", "tensor_copy", [b_halo], [b_upb], out=upb[:, 0:2], in_=halo[:, ch, :])
                    OP("act", "activation", [pu[1]], [b_upb], out=upb[:, 2:ST + 2], in_=pu[0][:, 0:ST], func=ACTF.Copy)
                    ps.free(pu)
                    OP("pool", "tensor_copy", [b_upb], [b_halo], out=halo[:, ch, :], in_=upb[:, ST:ST + 2])
                    u, b_u = u_r.next()
                    OP("act", "activation", [b_upb, b_cw, b_cb], [b_u], out=u[:], in_=upb[:, 2:ST + 2], func=ACTF.Identity,
                       scale=cw[:, 2, ch:ch + 1], bias=cb[:, ch:ch + 1])
                    OP("dve", "scalar_tensor_tensor", [b_upb, b_cw, b_u], [b_u], out=u[:], in0=upb[:, 1:ST + 1], scalar=cw[:, 1, ch:ch + 1], in1=u[:],
                       op0=ALU.mult, op1=ALU.add)
                    OP("dve", "scalar_tensor_tensor", [b_upb, b_cw, b_u], [b_u], out=u[:], in0=upb[:, 0:ST], scalar=cw[:, 0, ch:ch + 1], in1=u[:],
                       op0=ALU.mult, op1=ALU.add)
                    return u, b_u

                def C_super(s):
                    t0 = s * ST
                    xtiles = []
                    xT, b_xT = xT_r.next()
                    for tt in range(TPS):
                        xs, b_xs = xs_r.next()
                        P.dma("sp", xs[:], X1_d[t0 + tt * 128:t0 + (tt + 1) * 128, :], writes=[b_xs], group=f"xs{(s * TPS + tt) % (2 * TPS)}")
                        xtiles.append((xs, b_xs))
                        for half in range(2):
                            pb = ps.alloc()
                            for q in range(4):
                                kc = half * 4 + q
                                OP("pe", "transpose", [b_xs, b_identF], [pb[1]], out=pb[0][:, q * 128:(q + 1) * 128],
                                   in_=xs[:, kc * 128:(kc + 1) * 128], identity=identF[:])
                            dst = AP(xT, half * 4 * ST + tt * 128, [[KC * ST, 128], [ST, 4], [1, 128]])
                            src = AP(pb[0], 0, [[512, 128], [128, 4], [1, 128]])
                            if half == 0:
                                OP("act", "activation", [pb[1]], [b_xT], out=dst, in_=src, func=ACTF.Copy)
                            else:
                                OP("dve", "tensor_copy", [pb[1]], [b_xT], out=dst, in_=src)
                            ps.free(pb)
                    gT, b_gT = gT_r.next()
                    for j in range(NFC):
                        ug, b_ug = conv_chunk(j, xT, b_xT)
                        uv, b_uv = conv_chunk(NFC + j, xT, b_xT)
                        sl, b_sl = sl_r.next()
                        OP("act", "activation", [b_ug], [b_sl], out=sl[:], in_=ug[:], func=ACTF.Silu)
                        OP("dve", "tensor_tensor", [b_sl, b_uv], [b_gT], out=gT[:, j, :], in0=sl[:], in1=uv[:], op=ALU.mult)
                    for tt in range(TPS):
                        xs, b_xs = xtiles[tt]
                        yres, b_yres = yres_r.next()
                        for nb in range(2):
                            pm = ps.alloc()
                            for j in range(NFC):
                                MM(pm[0][:], gT[:, j, tt * 128:(tt + 1) * 128], wDn[:, j, nb * 512:(nb + 1) * 512], j == 0, j == NFC - 1,
                                   [b_gT, b_wDn[j]], [pm[1]])
                            OP("dve", "scalar_tensor_tensor", [b_xs, pm[1]], [b_yres], out=yres[:, nb * 512:(nb + 1) * 512],
                               in0=xs[:, nb * 512:(nb + 1) * 512], scalar=ALPHA, in1=pm[0][:], op0=ALU.mult, op1=ALU.add)
                            ps.free(pm)
                        ot, b_ot = o_r.next()
                        st, b_st = st_r.next()
                        layer_norm(yres, b_yres, ot, b_ot, g2B, b_g2B, b2B, b_b2B, st, b_st, nhalf, b_nhalf)
                        P.dma("pool", out_d[t0 + tt * 128:t0 + (tt + 1) * 128, :], ot[:], reads=[b_ot], group=f"ost{(s * TPS + tt) % 2}")

                for s in range(NS if "C" in phases else 0):
                    C_super(s)
                P.emit(E)
                P.barrier(("sp",))
                stats["C"] = (len(P.all_ops), P.n_wait)
    return nc, stats


def host_consts(L):
    idx = np.arange(128)
    ident = np.eye(128, dtype=np.float32)
    le = (idx[:, None] <= idx[None, :])
    uneg = np.where(le, -1.0 / 16.0, 0.0).astype(np.float32)
    lneg = np.where(idx[:, None] > idx[None, :], -1.0 / 16.0, 0.0).astype(np.float32)
    u01 = le.astype(np.float32)
    cm = np.where(idx[None, :] <= idx[:, None], 0.0, NEG_BIG).astype(np.float32)
    pos = np.arange(L, dtype=np.float32)

    def tab(rot):
        half = rot // 2
        inv = (np.float32(500000.0) ** (-np.arange(half, dtype=np.float32) * np.float32(2.0) / np.float32(rot))).astype(np.float32)
        ang = (pos[:, None] * inv[None, :]).astype(np.float32)
        c = np.cos(ang).astype(np.float32)
        s = np.sin(ang).astype(np.float32)
        return np.concatenate([c, c, -s, s], axis=1)

    rope = np.concatenate([tab(16), tab(8)], axis=1).astype(np.float32)
    pow2 = np.tile((2.0 ** -(np.arange(16, dtype=np.float32) + 1.0))[None, :], (128, 1)).astype(np.float32)
    return dict(c_ident=ident, c_uneg=uneg, c_lneg=lneg, c_u01=u01, c_cm=cm, c_rope=rope, c_pow2=pow2)


def make_in_maps(inputs, L, n_cores):
    f = lambda a: np.ascontiguousarray(np.asarray(a, dtype=np.float32))
    consts = host_consts(L)
    shared = dict(
        w_in=f(np.asarray(inputs["w_in"])[0][:, W_IN_PERM]),
        w_a2=f(inputs["w_gla_a2"][0]),
        b_a=f(np.asarray(inputs["b_gla_a"])[0][None, :]),
        gla_gain=f(np.asarray(inputs["gla_norm_gain"])[0].reshape(1, 512)),
        w_br=f(np.asarray(inputs["w_branch"])[0].reshape(1024, D)),
        w_o=f(inputs["w_o"][0]),
        ln1g=f(np.asarray(inputs["ln1_gain"])[0][None, :]),
        ln1b=f(np.asarray(inputs["ln1_bias"])[0][None, :]),
        w_up=f(inputs["w_up"][0]),
        conv_w=f(inputs["conv_w"][0]),
        conv_b=f(np.asarray(inputs["conv_b"])[0][None, :]),
        w_down=f(inputs["w_down"][0]),
        ln2g=f(np.asarray(inputs["ln2_gain"])[0][None, :]),
        ln2b=f(np.asarray(inputs["ln2_bias"])[0][None, :]),
        **consts,
    )
    x = np.asarray(inputs["x"], dtype=np.float32)
    return [dict(shared, x=np.ascontiguousarray(x[b])) for b in range(n_cores)]


_CACHE = {}


def kernel(**inputs):
    x = np.asarray(inputs["x"])
    B, L, _ = x.shape
    topk = min(256, L // 4)
    key = (L, topk)
    if key not in _CACHE:
        _CACHE[key] = build(L, topk)[0]
    nc = _CACHE[key]
    in_maps = make_in_maps(inputs, L, B)
    res = run_bass_kernel_spmd(nc, in_maps, core_ids=list(range(B)))
    return np.stack([np.asarray(r["out"], dtype=np.float32) for r in res.results], axis=0)
```

```python
import math
from contextlib import ExitStack
import numpy as np
import concourse.bass as bass
import concourse.mybir as mybir
from concourse.bass_utils import run_bass_kernel_spmd

F32 = mybir.dt.float32
BF16 = mybir.dt.bfloat16
U8 = mybir.dt.uint8
ALU = mybir.AluOpType
ACTF = mybir.ActivationFunctionType
AX = mybir.AxisListType

D = 1024
KC = 8
D_FF = 2816
NFC = D_FF // 128
ALPHA = 2.0 ** 0.25
LN_EPS = 1e-5
RMS_EPS = 1e-6
NEG_BIG = -1.0e30
MASK_NEG = -30000.0

ENGINES = ("pe", "act", "dve", "pool", "sp")
SEM_CHUNK = 60000


class Buf:
    __slots__ = ("name", "last_w", "readers", "excl")

    def __init__(self, name, excl=False):
        self.name = name
        self.last_w = None
        self.readers = []
        self.excl = excl


class Op:
    __slots__ = ("eng", "fn", "deps", "signal", "dma_group", "dma_cum")

    def __init__(self, eng, fn):
        self.eng = eng
        self.fn = fn
        self.deps = []
        self.signal = False
        self.dma_group = None
        self.dma_cum = 0


class Prog:
    def __init__(self, nc, tag):
        self.nc = nc
        self.tag = tag
        self.ops = {e: [] for e in ENGINES}
        self.all_ops = []
        self.dma_groups = {}
        self.eng_obj = {"pe": nc.tensor, "act": nc.scalar, "dve": nc.vector, "pool": nc.gpsimd, "sp": nc.sync}
        self.finals = []

    def buf(self, name):
        return Buf(name)

    def op(self, eng, fn, reads=(), writes=()):
        o = Op(eng, fn)
        for b in reads:
            if b.last_w is not None:
                o.deps.append((b.last_w, 0))
            if b.excl:
                for r in b.readers:
                    if r.eng != eng:
                        o.deps.append((r, 0))
        for b in writes:
            if b.last_w is not None:
                o.deps.append((b.last_w, 0))
            for r in b.readers:
                o.deps.append((r, 1))
        for b in reads:
            b.readers.append(o)
        for b in writes:
            b.last_w = o
            b.readers = []
        self.ops[eng].append(o)
        self.all_ops.append(o)
        return o

    def dma(self, eng, out, in_, reads=(), writes=(), group=None, mode="seq", noncontig=False):
        if noncontig:
            def fn(e):
                with self.nc.allow_non_contiguous_dma(reason="tiny strided parameter load"):
                    return e.dma_start(out=out, in_=in_)
        else:
            def fn(e):
                return e.dma_start(out=out, in_=in_)
        o = self.op(eng, fn, reads, writes)
        g = self.dma_groups.setdefault(group, {"mode": mode, "count": 0})
        g["count"] += 1
        o.dma_group = group
        o.dma_cum = g["count"]
        return o

    def emit(self, E):
        nc = self.nc
        for o in self.all_ops:
            kept = []
            for (d, war) in o.deps:
                if d.dma_group is None and d.eng == o.eng and (war or o.eng == "pe"):
                    continue
                kept.append(d)
                d.signal = True
            o.deps = kept
        eng_sems = {}
        sig_count = {}
        for e in ENGINES:
            comp = [o for o in self.ops[e] if o.dma_group is None]
            if comp:
                comp[-1].signal = True
            n = 0
            for o in comp:
                if o.signal:
                    n += 1
                    sig_count[o] = n
            nsem = (n + SEM_CHUNK - 1) // SEM_CHUNK
            eng_sems[e] = [E(nc.semaphore(f"s{self.tag}_{e}_{k}")) for k in range(nsem)]
            if n:
                k, v = divmod(n - 1, SEM_CHUNK)
                self.finals.append((eng_sems[e][k], v + 1))
        dma_sems = {g: E(nc.semaphore(f"d{self.tag}_{g}")) for g in self.dma_groups}
        for g, info in self.dma_groups.items():
            self.finals.append((dma_sems[g], 16 * info["count"]))

        def target(d, o=None):
            if d.dma_group is not None:
                g = self.dma_groups[d.dma_group]
                assert not (g["mode"] == "all" and o is not None and o.dma_group == d.dma_group), "self-deadlock in 'all' dma group"
                cnt = g["count"] if g["mode"] == "all" else d.dma_cum
                return ("dma", d.dma_group), dma_sems[d.dma_group], 16 * cnt
            k, v = divmod(sig_count[d] - 1, SEM_CHUNK)
            return (d.eng, k), eng_sems[d.eng][k], v + 1

        n_wait = 0
        for e in ENGINES:
            eo = self.eng_obj[e]
            waited = {}
            eng_hi = {}
            for o in self.ops[e]:
                need = {}
                for d in o.deps:
                    key, sem, val = target(d, o)
                    if key[0] != "dma" and eng_hi.get(key[0], -1) > key[1]:
                        continue
                    if waited.get(key, 0) >= val:
                        continue
                    if key not in need or need[key][1] < val:
                        need[key] = (sem, val)
                for key, (sem, val) in need.items():
                    eo.wait_ge(sem, val)
                    n_wait += 1
                    waited[key] = val
                    if key[0] != "dma":
                        eng_hi[key[0]] = max(eng_hi.get(key[0], -1), key[1])
                ins = o.fn(eo)
                if o.dma_group is not None:
                    ins.then_inc(dma_sems[o.dma_group], 16)
                elif o.signal:
                    k, v = divmod(sig_count[o] - 1, SEM_CHUNK)
                    ins.then_inc(eng_sems[e][k], 1)
        self.n_wait = n_wait

    def barrier(self, engines=ENGINES):
        for e in engines:
            eo = self.eng_obj[e]
            for sem, val in self.finals:
                eo.wait_ge(sem, val)


class PsumPool:
    def __init__(self, nc, P, EP, n=8):
        self.free_list = []
        for k in range(n):
            t = EP(nc.psum_tensor(f"pb{P.tag}{k}", [128, 512], F32))
            self.free_list.append((t, Buf(f"pb{k}", excl=True)))

    def alloc(self):
        assert self.free_list, "PSUM pool exhausted"
        return self.free_list.pop(0)

    def free(self, item):
        self.free_list.append(item)


class Rot:
    def __init__(self, nc, P, EP, name, shape, dtype, n):
        self.items = [(EP(nc.sbuf_tensor(f"{name}{P.tag}{k}", shape, dtype)), P.buf(f"{name}{k}")) for k in range(n)]
        self.i = 0

    def next(self):
        it = self.items[self.i % len(self.items)]
        self.i += 1
        return it


def AP(t, off, pat):
    return bass.AP(t, off, [list(p) for p in pat])


_O = dict(gq=0, gk=256, gv=512, ga=1024, gr=1040, dq=1552, dk=2064, dv=2128, iq=2192, ik=2448, iw=2480, gate=2488)
_W = dict(gq=256, gk=256, gv=512, ga=16, gr=512, dq=512, dk=64, dv=64, iq=256, ik=32, iw=8, gate=2048)
_ORDER = ["gv", "gr", "dq", "gk", "iq", "dk", "dv", "ik", "iw", "ga", "gq", "gate"]
_C = {}
_off = 0
for _n in _ORDER:
    _C[_n] = _off
    _off += _W[_n]
W_IN_PERM = np.concatenate([np.arange(_O[n], _O[n] + _W[n]) for n in _ORDER])
NCOLA = _C["gate"]
MISC0 = _C["dk"]
MISCW = 184


def build(L, topk, nbis=10, debug=False, phases="ABC", stages=4, cut=99):
    NT = L // 128
    nc = bass.Bass("TRN2", target_bir_lowering=False)

    def din(name, shape, dt=F32):
        return nc.dram_tensor(name, list(shape), dt, kind="ExternalInput").ap()

    x_d = din("x", [L, D])
    w_in_d = din("w_in", [D, 4536])
    w_a2_d = din("w_a2", [16, 256])
    b_a_d = din("b_a", [1, 256])
    gain_d = din("gla_gain", [1, 512])
    w_br_d = din("w_br", [1024, D])
    w_o_d = din("w_o", [D, D])
    ln1g_d = din("ln1g", [1, D])
    ln1b_d = din("ln1b", [1, D])
    w_up_d = din("w_up", [D, 2 * D_FF])
    cw_d = din("conv_w", [3, 2 * D_FF])
    cb_d = din("conv_b", [1, 2 * D_FF])
    w_dn_d = din("w_down", [D_FF, D])
    ln2g_d = din("ln2g", [1, D])
    ln2b_d = din("ln2b", [1, D])
    c_ident = din("c_ident", [128, 128])
    c_uneg = din("c_uneg", [128, 128])
    c_lneg = din("c_lneg", [128, 128])
    c_u01 = din("c_u01", [128, 128])
    c_cm = din("c_cm", [128, 128])
    c_rope = din("c_rope", [L, 48])
    c_pow2 = din("c_pow2", [128, 16])
    out_d = nc.dram_tensor("out", [L, D], F32, kind="ExternalOutput").ap()
    Y_d = nc.dram_tensor("Y_scr", [L, D], BF16, kind="Internal").ap()
    X1_d = nc.dram_tensor("X1_scr", [L, D], F32, kind="Internal").ap()
    dbg = {}
    if debug:
        dbg["y"] = nc.dram_tensor("dbg_y", [L, D], F32, kind="ExternalOutput").ap()
        dbg["x1"] = nc.dram_tensor("dbg_x1", [L, D], F32, kind="ExternalOutput").ap()
        dbg["S"] = nc.dram_tensor("dbg_S", [L, L], F32, kind="ExternalOutput").ap()
        dbg["th"] = nc.dram_tensor("dbg_th", [L, 2], F32, kind="ExternalOutput").ap()

    stats = {}
    with ExitStack() as outer:
        E = outer.enter_context

        if "A" in phases:
            with ExitStack() as ph:
                EP = ph.enter_context
                P = Prog(nc, "A")

                def T(name, shape, dt):
                    return EP(nc.sbuf_tensor(name + "A", list(shape), dt)), P.buf(name)

                def OP(eng, method, reads=(), writes=(), **kw):
                    return P.op(eng, lambda e: getattr(e, method)(**kw), reads, writes)

                def MM(out, lhsT, rhs, start, stop, reads, writes, **kw):
                    return P.op("pe", lambda e: e.matmul(out, lhsT=lhsT, rhs=rhs, start=start, stop=stop, **kw), reads, writes)

                ps = PsumPool(nc, P, EP)
                identF, b_identF = T("identF", [128, 128], F32)
                identB, b_identB = T("identB", [128, 128], BF16)
                uneg, b_uneg = T("uneg", [128, 128], F32)
                lneg, b_lneg = T("lneg", [128, 128], F32)
                u01f, b_u01f = T("u01f", [128, 128], F32)
                u01x4, b_u01x4 = T("u01x4", [128, 512], F32)
                cm, b_cm = T("cm", [128, 128], F32)
                I4, b_I4 = T("I4", [128, 512], BF16)
                pow2, b_pow2 = T("pow2", [128, 16], F32)
                ones1, b_ones1 = T("ones1", [1, 128], F32)
                b_a, b_b_a = T("b_a", [1, 256], F32)
                w_a2, b_w_a2 = T("w_a2", [16, 256], F32)
                gainB, b_gainB = T("gainB", [128, 512], F32)
                nhalf, b_nhalf = T("nhalf", [128, 8], F32)
                thneg, b_thneg = T("thneg", [128, 1], F32)
                P.dma("sp", identF[:], c_ident, writes=[b_identF], group="c", mode="all")
                P.dma("sp", uneg[:], c_uneg, writes=[b_uneg], group="c", mode="all")
                P.dma("sp", lneg[:], c_lneg, writes=[b_lneg], group="c", mode="all")
                P.dma("sp", u01f[:], c_u01, writes=[b_u01f], group="c", mode="all")
                P.dma("sp", cm[:], c_cm, writes=[b_cm], group="c", mode="all")
                P.dma("sp", pow2[:], c_pow2, writes=[b_pow2], group="c", mode="all")
                P.dma("sp", b_a[:], b_a_d, writes=[b_b_a], group="c", mode="all")
                P.dma("sp", w_a2[:], w_a2_d, writes=[b_w_a2], group="c", mode="all")
                P.dma("sp", gainB[:], AP(gain_d.tensor, 0, [[0, 128], [1, 512]]), writes=[b_gainB], group="c", mode="all")
                OP("dve", "tensor_copy", [b_identF], [b_identB], out=identB[:], in_=identF[:])
                for q in range(4):
                    OP("dve", "tensor_copy", [b_identF], [b_I4], out=I4[:, q * 128:(q + 1) * 128], in_=identF[:])
                    OP("dve", "tensor_copy", [b_u01f], [b_u01x4], out=u01x4[:, q * 128:(q + 1) * 128], in_=u01f[:])
                OP("dve", "memset", [], [b_ones1], ap=ones1[:], constant=1.0)
                OP("dve", "memset", [], [b_nhalf], ap=nhalf[:], constant=-0.5)
                OP("dve", "memset", [], [b_thneg], ap=thneg[:], constant=-1.0e29)
                wA = EP(nc.sbuf_tensor("wA", [128, KC, NCOLA], BF16))
                b_wA = [P.buf(f"wA{k}") for k in range(KC)]
                for kc in range(KC):
                    P.dma("pool", wA[:, kc, :], w_in_d[kc * 128:(kc + 1) * 128, 0:NCOLA], writes=[b_wA[kc]], group=f"wA{kc}")
                kTa, b_kTa = T("kTa", [128, L], BF16)
                kiT, b_kiT = T("kiT", [128, L], BF16)
                vaug, b_vaug = T("vaug", [128, NT, 65], BF16)
                S_row, b_S = T("S_row", [128, L], F32)
                junk8, b_junk8 = T("junk8", [128, L], U8)
                state, b_state = T("state", [128, 256], F32)
                state_bf, b_state_bf = T("state_bf", [128, 256], BF16)
                OP("pool", "memset", [], [b_kTa], ap=kTa[64:65, :], constant=1.0)
                OP("pool", "memset", [], [b_vaug], ap=vaug[:], constant=1.0)
                OP("pool", "memset", [], [b_state], ap=state[:], constant=0.0)
                OP("pool", "memset", [], [b_state_bf], ap=state_bf[:], constant=0.0)
                xs_r = Rot(nc, P, EP, "xs", [128, D], F32, 2)
                rp_r = Rot(nc, P, EP, "rp", [128, 48], F32, 2)
                xT_r = Rot(nc, P, EP, "xT", [128, KC, 128], BF16, 2)
                misc_r = Rot(nc, P, EP, "misc", [128, MISCW], F32, 2)
                gkiq_r = Rot(nc, P, EP, "gkiq", [128, 512], F32, 1)
                dqf_r = Rot(nc, P, EP, "dqf", [128, 512], F32, 1)
                vtok_r = Rot(nc, P, EP, "vtok", [128, 512], BF16, 2)
                rf_r = Rot(nc, P, EP, "rf", [128, 512], F32, 1)
                re_r = Rot(nc, P, EP, "re", [128, 512], F32, 1)
                G_r = Rot(nc, P, EP, "G", [128, 512], F32, 1)
                alT_r = Rot(nc, P, EP, "alT", [16, 128], F32, 1)
                e1_r = Rot(nc, P, EP, "e1", [128, 256], F32, 1)
                l_r = Rot(nc, P, EP, "l", [128, 256], F32, 1)
                ebT_r = Rot(nc, P, EP, "ebT", [128, 256], F32, 1)
                enbT_r = Rot(nc, P, EP, "enbT", [128, 256], F32, 1)
                ed_r = Rot(nc, P, EP, "ed", [128, 256], F32, 1)
                qz_r = Rot(nc, P, EP, "qz", [128, 4, 128], BF16, 2)
                ktil_r = Rot(nc, P, EP, "ktil", [128, 256], BF16, 2)
                khat_r = Rot(nc, P, EP, "khat", [128, 256], BF16, 2)
                ATm_r = Rot(nc, P, EP, "ATm", [128, 512], BF16, 2)
                ssq_r = Rot(nc, P, EP, "ssq", [128, 8], F32, 2)
                sqj_r = Rot(nc, P, EP, "sqj", [128, 128], F32, 1)
                ytile_r = Rot(nc, P, EP, "ytile", [128, D], BF16, 2)
                rt_r = Rot(nc, P, EP, "rt", [128, 8, 32], F32, 1)
                q_r_r = Rot(nc, P, EP, "q_r", [128, 8, 64], BF16, 2)
                k_r_r = Rot(nc, P, EP, "k_r", [128, 96], BF16, 2)
                qi_r_r = Rot(nc, P, EP, "qi_r", [128, 8, 32], BF16, 2)
                qTa_r = Rot(nc, P, EP, "qTa", [128, 1024], BF16, 2)
                qiT2_r = Rot(nc, P, EP, "qiT2", [128, 1024], BF16, 2)
                WdT_r = Rot(nc, P, EP, "WdT", [128, 1024], BF16, 1)
                Wd_r = Rot(nc, P, EP, "Wd", [128, 8, 128], BF16, 2)
                R_r = Rot(nc, P, EP, "R", [128, 512], BF16, 5)
                NM_r = Rot(nc, P, EP, "NM", [128, 128], BF16, 4)
                PT_r = Rot(nc, P, EP, "PT", [128, 1024], BF16, 3)
                bst_r = Rot(nc, P, EP, "bst", [128, 32], F32, 2)
                bmid_r = Rot(nc, P, EP, "bmid", [128, 2], F32, 2)
                bcnt_r = Rot(nc, P, EP, "bcnt", [128, 2], F32, 2)
                bsgn_r = Rot(nc, P, EP, "bsgn", [128, 2], F32, 2)
                bu_r = Rot(nc, P, EP, "bu", [128, 2], F32, 2)
                b_junkD = P.buf("junkD")
                b_junkA = P.buf("junkA")
                rden_r = Rot(nc, P, EP, "rden", [128, 8], F32, 2)
                for (t_, b_) in qTa_r.items:
                    OP("pool", "memset", [], [b_], ap=t_[64:65, :], constant=0.0)
                for (t_, b_) in qz_r.items:
                    OP("pool", "memset", [], [b_], ap=t_[:], constant=0.0)
                for (t_, b_) in qiT2_r.items:
                    OP("pool", "memset", [], [b_], ap=t_[:], constant=0.0)
                OP("pool", "memset", [], [b_kiT], ap=kiT[:], constant=0.0)

                tile_ctx = {}

                def A_P1(i):
                    t0 = i * 128
                    xs, b_xs = xs_r.next()
                    P.dma("sp", xs[:], x_d[t0:t0 + 128, :], writes=[b_xs], group=f"xs{i % 2}")
                    rp, b_rp = rp_r.next()
                    P.dma("sp", rp[:], c_rope[t0:t0 + 128, :], writes=[b_rp], group=f"rp{i % 2}")
                    xT, b_xT = xT_r.next()
                    for half in range(2):
                        pb = ps.alloc()
                        for q in range(4):
                            kc = half * 4 + q
                            OP("pe", "transpose", [b_xs, b_identF], [pb[1]], out=pb[0][:, q * 128:(q + 1) * 128],
                               in_=xs[:, kc * 128:(kc + 1) * 128], identity=identF[:])
                        dst = AP(xT, half * 512, [[1024, 128], [1, 512]])
                        if half == 0:
                            OP("act", "activation", [pb[1]], [b_xT], out=dst, in_=pb[0][:], func=ACTF.Copy)
                        else:
                            OP("dve", "tensor_copy", [pb[1]], [b_xT], out=dst, in_=pb[0][:])
                        ps.free(pb)

                    def tokbank(c0, ncols):
                        pb = ps.alloc()
                        for kc in range(KC):
                            MM(pb[0][:, 0:ncols], xT[:, kc, :], wA[:, kc, c0:c0 + ncols], kc == 0, kc == KC - 1,
                               [b_xT, b_wA[kc]], [pb[1]])
                        return pb

                    yield
                    misc, b_misc = misc_r.next()
                    pb = tokbank(MISC0, MISCW)
                    OP("act", "activation", [pb[1]], [b_misc], out=misc[:], in_=pb[0][:, 0:MISCW], func=ACTF.Copy)
                    ps.free(pb)
                    yield
                    alT, b_alT = alT_r.next()
                    pb = ps.alloc()
                    MM(pb[0][0:16, 0:128], misc[:, 168:184], identF[:], True, True, [b_misc, b_identF], [pb[1]])
                    OP("act", "activation", [pb[1]], [b_alT], out=alT[:], in_=pb[0][0:16, 0:128], func=ACTF.Copy)
                    ps.free(pb)
                    pz = ps.alloc()
                    MM(pz[0][:, 0:256], ones1[0:1, :], b_a[0:1, :], True, False, [b_ones1, b_b_a], [pz[1]])
                    MM(pz[0][:, 0:256], alT[0:16, :], w_a2[0:16, :], False, True, [b_alT, b_w_a2], [pz[1]])
                    e1, b_e1 = e1_r.next()
                    lt, b_l = l_r.next()
                    OP("act", "activation", [pz[1]], [b_e1], out=e1[:], in_=pz[0][:, 0:256], func=ACTF.Exp, scale=-1.0)
                    ps.free(pz)
                    OP("act", "activation", [b_e1], [b_l], out=lt[:], in_=e1[:], func=ACTF.Ln, bias=1.0)
                    pc = ps.alloc()
                    for p in range(2):
                        MM(pc[0][:, p * 128:(p + 1) * 128], lt[:, p * 128:(p + 1) * 128], uneg[:], True, True, [b_l, b_uneg], [pc[1]])
                    MM(pc[0][:, 256:512], lneg[:], lt[:, 0:256], True, True, [b_l, b_lneg], [pc[1]])
                    ebT, b_ebT = ebT_r.next()
                    enbT, b_enbT = enbT_r.next()
                    ed, b_ed = ed_r.next()
                    OP("act", "activation", [pc[1]], [b_ebT], out=ebT[:], in_=pc[0][:, 0:256], func=ACTF.Exp)
                    OP("act", "activation", [pc[1]], [b_enbT], out=enbT[:], in_=pc[0][:, 0:256], func=ACTF.Exp, scale=-1.0)
                    OP("act", "activation", [pc[1]], [b_ed], out=ed[:], in_=pc[0][:, 256:512], func=ACTF.Exp)
                    ps.free(pc)
                    yield
                    gkiq, b_gkiq = gkiq_r.next()
                    pb = tokbank(_C["gk"], 512)
                    OP("dve", "tensor_copy", [pb[1]], [b_gkiq], out=gkiq[:], in_=pb[0][:])
                    ps.free(pb)
                    vtok, b_vtok = vtok_r.next()
                    pb = tokbank(_C["gv"], 512)
                    OP("act", "activation", [pb[1]], [b_vtok], out=vtok[:], in_=pb[0][:], func=ACTF.Copy)
                    ps.free(pb)
                    yield
                    rf, b_rf = rf_r.next()
                    re_, b_re = re_r.next()
                    G, b_G = G_r.next()
                    pb = tokbank(_C["gr"], 512)
                    OP("dve", "tensor_copy", [pb[1]], [b_rf], out=rf[:], in_=pb[0][:])
                    OP("act", "activation", [pb[1]], [b_re], out=re_[:], in_=pb[0][:], func=ACTF.Exp, scale=-1.0)
                    ps.free(pb)
                    yield
                    OP("dve", "tensor_scalar", [b_re], [b_re], out=re_[:], in0=re_[:], scalar1=1.0, scalar2=None, op0=ALU.add)
                    OP("dve", "reciprocal", [b_re], [b_re], out=re_[:], in_=re_[:])
                    yield
                    OP("pool", "tensor_tensor", [b_rf, b_re], [b_G], out=G[:], in0=rf[:], in1=re_[:], op=ALU.mult)
                    OP("pool", "tensor_tensor", [b_G, b_gainB], [b_G], out=G[:], in0=G[:], in1=gainB[:], op=ALU.mult)
                    yield
                    dqf, b_dqf = dqf_r.next()
                    pb = tokbank(_C["dq"], 512)
                    OP("act", "activation", [pb[1]], [b_dqf], out=dqf[:], in_=pb[0][:], func=ACTF.Copy)
                    ps.free(pb)
                    yield
                    pf = ps.alloc()
                    for j in range(4):
                        c0 = (_C["gq"] + j * 128) if j < 2 else (_C["gk"] + (j - 2) * 128)
                        for kc in range(KC):
                            MM(pf[0][:, j * 128:(j + 1) * 128], wA[:, kc, c0:c0 + 128], xT[:, kc, :], kc == 0, kc == KC - 1,
                               [b_xT, b_wA[kc]], [pf[1]])
                    qz, b_qz = qz_r.next()
                    ktil, b_ktil = ktil_r.next()
                    khat, b_khat = khat_r.next()
                    for h in range(4):
                        p, hh = divmod(h, 2)
                        OP("dve", "scalar_tensor_tensor", [pf[1], b_ebT], [b_qz], out=qz[hh * 64:(hh + 1) * 64, h, :],
                           in0=pf[0][hh * 64:(hh + 1) * 64, p * 128:(p + 1) * 128], scalar=0.125,
                           in1=ebT[hh * 64:(hh + 1) * 64, p * 128:(p + 1) * 128], op0=ALU.mult, op1=ALU.mult)
                    OP("dve", "tensor_tensor", [pf[1], b_enbT], [b_ktil], out=ktil[:], in0=pf[0][:, 256:512], in1=enbT[:], op=ALU.mult)
                    ps.free(pf)
                    OP("pool", "tensor_tensor", [b_gkiq, b_ed], [b_khat], out=khat[:], in0=gkiq[:, 0:256], in1=ed[:], op=ALU.mult)
                    yield
                    pa = ps.alloc()
                    for h in range(4):
                        p, hh = divmod(h, 2)
                        MM(pa[0][:, h * 128:(h + 1) * 128], ktil[:, p * 128:(p + 1) * 128],
                           qz[:, h, :], True, True, [b_ktil, b_qz], [pa[1]])
                    ATm, b_ATm = ATm_r.next()
                    OP("dve", "tensor_tensor", [pa[1], b_u01x4], [b_ATm], out=ATm[:], in0=pa[0][:], in1=u01x4[:], op=ALU.mult)
                    ps.free(pa)
                    po = ps.alloc()
                    for h in range(4):
                        p, hh = divmod(h, 2)
                        MM(po[0][:, h * 128:(h + 1) * 128], qz[:, h, :],
                           state_bf[:, p * 128:(p + 1) * 128], True, False, [b_qz, b_state_bf], [po[1]])
                        MM(po[0][:, h * 128:(h + 1) * 128], ATm[:, h * 128:(h + 1) * 128], vtok[:, h * 128:(h + 1) * 128],
                           False, True, [b_ATm, b_vtok], [po[1]])
                    yield
                    pu = ps.alloc()
                    for p in range(2):
                        MM(pu[0][:, p * 256:(p + 1) * 256], khat[:, p * 128:(p + 1) * 128], vtok[:, p * 256:(p + 1) * 256], True, True,
                           [b_khat, b_vtok], [pu[1]])
                    for p in range(2):
                        for hh in range(2):
                            OP("dve", "scalar_tensor_tensor", [b_state, b_ebT, pu[1]], [b_state],
                               out=state[hh * 64:(hh + 1) * 64, p * 128:(p + 1) * 128],
                               in0=state[hh * 64:(hh + 1) * 64, p * 128:(p + 1) * 128],
                               scalar=ebT[hh * 64:(hh + 1) * 64, p * 128 + 127:p * 128 + 128],
                               in1=pu[0][hh * 64:(hh + 1) * 64, p * 256 + hh * 128:p * 256 + hh * 128 + 128],
                               op0=ALU.mult, op1=ALU.add)
                    ps.free(pu)
                    OP("pool", "tensor_copy", [b_state], [b_state_bf], out=state_bf[:], in_=state[:])
                    yield
                    ssq, b_ssq = ssq_r.next()
                    sqj, b_sqj = sqj_r.next()
                    for h in range(4):
                        OP("act", "activation", [po[1]], [b_sqj, b_ssq], out=sqj[:], in_=po[0][:, h * 128:(h + 1) * 128], func=ACTF.Square,
                           accum_out=ssq[:, h:h + 1])
                    OP("dve", "tensor_scalar", [b_ssq], [b_ssq], out=ssq[:, 4:8], in0=ssq[:, 0:4], scalar1=1.0 / 128.0, scalar2=RMS_EPS,
                       op0=ALU.mult, op1=ALU.add)
                    OP("pool", "tensor_tensor", [b_ssq, b_nhalf], [b_ssq], out=ssq[:, 0:4], in0=ssq[:, 4:8], in1=nhalf[:, 0:4], op=ALU.pow)
                    ytile, b_ytile = ytile_r.next()
                    for h in range(4):
                        OP("dve", "scalar_tensor_tensor", [po[1], b_ssq, b_G], [b_ytile], out=ytile[:, h * 128:(h + 1) * 128],
                           in0=po[0][:, h * 128:(h + 1) * 128], scalar=ssq[:, h:h + 1], in1=G[:, h * 128:(h + 1) * 128],
                           op0=ALU.mult, op1=ALU.mult)
                    ps.free(po)

                    yield
                    rt, b_rt = rt_r.next()
                    q_r, b_q_r = q_r_r.next()
                    k_r, b_k_r = k_r_r.next()
                    qi_r, b_qi_r = qi_r_r.next()

                    def rope(src_t, src_off, src_pstride, nh, hd, half, tab0, dst_t, dst_off, dst_pstride, rd, wr):
                        rot = 2 * half
                        sA = AP(src_t, src_off, [[src_pstride, 128], [hd, nh], [1, rot]])
                        cc = AP(rp, tab0, [[48, 128], [0, nh], [1, rot]])
                        tA = AP(rt, 0, [[256, 128], [16, nh], [1, rot]])
                        tB = AP(rt, 128, [[256, 128], [16, nh], [1, rot]])
                        OP("pool", "tensor_tensor", rd + [b_rp], [b_rt], out=tA, in0=sA, in1=cc, op=ALU.mult)
                        s2 = AP(src_t, src_off + half, [[src_pstride, 128], [hd, nh], [1, half]])
                        s1 = AP(src_t, src_off, [[src_pstride, 128], [hd, nh], [1, half]])
                        nsin = AP(rp, tab0 + rot, [[48, 128], [0, nh], [1, half]])
                        psin = AP(rp, tab0 + rot + half, [[48, 128], [0, nh], [1, half]])
                        tB1 = AP(rt, 128, [[256, 128], [16, nh], [1, half]])
                        tB2 = AP(rt, 128 + half, [[256, 128], [16, nh], [1, half]])
                        OP("pool", "tensor_tensor", rd + [b_rp], [b_rt], out=tB1, in0=s2, in1=nsin, op=ALU.mult)
                        OP("pool", "tensor_tensor", rd + [b_rp], [b_rt], out=tB2, in0=s1, in1=psin, op=ALU.mult)
                        dR = AP(dst_t, dst_off, [[dst_pstride, 128], [hd, nh], [1, rot]])
                        OP("pool", "tensor_tensor", [b_rt], wr, out=dR, in0=tA, in1=tB, op=ALU.add)
                        sN = AP(src_t, src_off + rot, [[src_pstride, 128], [hd, nh], [1, hd - rot]])
                        dN = AP(dst_t, dst_off + rot, [[dst_pstride, 128], [hd, nh], [1, hd - rot]])
                        OP("pool", "tensor_copy", rd, wr, out=dN, in_=sN)

                    rope(dqf, 0, 512, 8, 64, 8, 0, q_r, 0, 512, [b_dqf], [b_q_r])
                    rope(misc, 0, MISCW, 1, 64, 8, 0, k_r, 0, 96, [b_misc], [b_k_r])
                    rope(gkiq, 256, 512, 8, 32, 4, 32, qi_r, 0, 256, [b_gkiq], [b_qi_r])
                    rope(misc, 128, MISCW, 1, 32, 4, 32, k_r, 64, 96, [b_misc], [b_k_r])
                    OP("pool", "tensor_copy", [b_misc], [b_vaug], out=vaug[:, i, 0:64], in_=misc[:, 64:128])
                    yield
                    qTa, b_qTa = qTa_r.next()
                    for b2 in range(2):
                        pq = ps.alloc()
                        for hh in range(4):
                            h = b2 * 4 + hh
                            MM(pq[0][0:64, hh * 128:(hh + 1) * 128], q_r[:, h, :], identB[:], True, True, [b_q_r, b_identB], [pq[1]])
                        OP("act", "activation", [pq[1]], [b_qTa], out=qTa[0:64, b2 * 512:(b2 + 1) * 512], in_=pq[0][0:64, :],
                           func=ACTF.Copy, scale=0.125)
                        ps.free(pq)
                    pk = ps.alloc()
                    MM(pk[0][0:64, 0:128], k_r[:, 0:64], identB[:], True, True, [b_k_r, b_identB], [pk[1]])
                    MM(pk[0][0:32, 128:256], k_r[:, 64:96], identB[:], True, True, [b_k_r, b_identB], [pk[1]])
                    OP("dve", "tensor_copy", [pk[1]], [b_kTa], out=kTa[0:64, t0:t0 + 128], in_=pk[0][0:64, 0:128])
                    OP("dve", "tensor_copy", [pk[1]], [b_kiT], out=kiT[0:32, t0:t0 + 128], in_=pk[0][0:32, 128:256])
                    ps.free(pk)
                    qiT2, b_qiT2 = qiT2_r.next()
                    for b2 in range(2):
                        pi_ = ps.alloc()
                        for hh in range(4):
                            h = b2 * 4 + hh
                            MM(pi_[0][0:32, hh * 128:(hh + 1) * 128], qi_r[:, h, :], identB[:], True, True, [b_qi_r, b_identB], [pi_[1]])
                        src = AP(pi_[0], 0, [[512, 32], [16, 8], [128, 4], [1, 16]])
                        dst = AP(qiT2, b2 * 64, [[1024, 32], [128, 8], [16, 4], [1, 16]])
                        OP("act", "activation", [pi_[1]], [b_qiT2], out=dst, in_=src, func=ACTF.Copy)
                        ps.free(pi_)
                    WdT, b_WdT = WdT_r.next()
                    in0 = AP(misc, 160, [[MISCW, 128], [0, 8], [1, 8], [0, 16]])
                    in1 = AP(identF, 0, [[128, 128], [16, 8], [0, 8], [1, 16]])
                    outw = AP(WdT, 0, [[1024, 128], [128, 8], [16, 8], [1, 16]])
                    OP("dve", "tensor_tensor", [b_misc, b_identF], [b_WdT], out=outw, in0=in0, in1=in1, op=ALU.mult)
                    Wd, b_Wd = Wd_r.next()
                    for b2 in range(2):
                        pw = ps.alloc()
                        for gg in range(4):
                            g = b2 * 4 + gg
                            MM(pw[0][:, gg * 128:(gg + 1) * 128], WdT[:, g * 128:(g + 1) * 128], identB[:], True, True, [b_WdT, b_identB], [pw[1]])
                        dst = AP(Wd, b2 * 512, [[1024, 128], [1, 512]])
                        if b2 == 0:
                            OP("dve", "tensor_copy", [pw[1]], [b_Wd], out=dst, in_=pw[0][:])
                        else:
                            OP("act", "activation", [pw[1]], [b_Wd], out=dst, in_=pw[0][:], func=ACTF.Copy)
                        ps.free(pw)
                    tile_ctx[i] = dict(qTa=(qTa, b_qTa), qiT2=(qiT2, b_qiT2), Wd=(Wd, b_Wd), ytile=(ytile, b_ytile))

                def A_P2(i):
                    t0 = i * 128
                    n_i = t0 + 128
                    qiT2, b_qiT2 = tile_ctx[i]["qiT2"]
                    Wd, b_Wd = tile_ctx[i]["Wd"]
                    nch = (n_i + 511) // 512
                    LA = 2
                    pend = []
                    pS_cur = {}

                    def score(item):
                        c5, g, w5, R, b_R = item
                        if g == 0:
                            pS_cur[c5] = ps.alloc()
                        pS = pS_cur[c5]
                        MM(pS[0][:, 0:w5], Wd[:, g, :], R[:, 0:w5], g == 0, g == 7, [b_Wd, b_R], [pS[1]])
                        if g == 7:
                            last = (c5 == nch - 1)
                            wplain = w5 - 128 if last else w5
                            if wplain > 0:
                                OP("act", "activation", [pS[1]], [b_S], out=S_row[:, c5 * 512:c5 * 512 + wplain], in_=pS[0][:, 0:wplain],
                                   func=ACTF.Copy)
                            if last:
                                OP("dve", "tensor_tensor", [pS[1], b_cm], [b_S], out=S_row[:, t0:t0 + 128], in0=pS[0][:, w5 - 128:w5],
                                   in1=cm[:], op=ALU.add)
                            ps.free(pS)

                    k = 0
                    for c5 in range(nch):
                        w5 = min(512, n_i - c5 * 512)
                        for g in range(8):
                            pL = ps.alloc()
                            MM(pL[0][:, 0:w5], qiT2[:, g * 128:(g + 1) * 128], kiT[:, c5 * 512:c5 * 512 + w5], True, True,
                               [b_qiT2, b_kiT], [pL[1]])
                            R, b_R = R_r.next()
                            if k % 2 == 0:
                                OP("act", "activation", [pL[1]], [b_R], out=R[:, 0:w5], in_=pL[0][:, 0:w5], func=ACTF.Relu)
                            else:
                                OP("dve", "tensor_scalar", [pL[1]], [b_R], out=R[:, 0:w5], in0=pL[0][:, 0:w5], scalar1=0.0, scalar2=None,
                                   op0=ALU.max)
                            k += 1
                            ps.free(pL)
                            pend.append((c5, g, w5, R, b_R))
                            if len(pend) > LA:
                                score(pend.pop(0))
                    while pend:
                        score(pend.pop(0))
                    if debug:
                        P.dma("sp", dbg["S"][t0:t0 + 128, 0:n_i], S_row[:, 0:n_i], reads=[b_S], group="dbgS")

                def A_P3(i):
                    t0 = i * 128
                    n_i = t0 + 128
                    if n_i <= topk:
                        tile_ctx[i]["theta"] = (thneg[:, 0:1], b_thneg)
                        return
                    a = int(round(0.45 * n_i / 128.0)) * 128
                    a = max(128, min(a, n_i - 128))
                    n_act = n_i - a
                    bst, b_st = bst_r.next()
                    bmid, b_mid = bmid_r.next()
                    bcnt, b_cnt = bcnt_r.next()
                    bsgn, b_sgn = bsgn_r.next()
                    bu, b_u = bu_r.next()
                    OP("dve", "tensor_reduce", [b_S], [b_st], out=bst[:, 0:1], in_=S_row[:, 0:n_i], axis=AX.X, op=ALU.max)
                    OP("dve", "tensor_reduce", [b_S, b_st], [b_st], out=bst[:, 1:2], in_=S_row[:, 0:t0], axis=AX.X, op=ALU.min)
                    OP("dve", "tensor_tensor", [b_st], [b_st], out=bst[:, 2:3], in0=bst[:, 0:1], in1=bst[:, 1:2], op=ALU.subtract)
                    OP("dve", "tensor_scalar", [b_st, b_pow2], [b_st], out=bst[:, 8:8 + nbis + 1], in0=pow2[:, 0:nbis + 1], scalar1=bst[:, 2:3],
                       scalar2=None, op0=ALU.mult)
                    OP("dve", "tensor_copy", [b_st], [b_st], out=bst[:, 8 + nbis:9 + nbis], in_=bst[:, 7 + nbis:8 + nbis])
                    OP("dve", "tensor_tensor", [b_st], [b_mid], out=bmid[:, 0:1], in0=bst[:, 1:2], in1=bst[:, 8:9], op=ALU.add)
                    for k in range(nbis):
                        dst = 0 if k < nbis - 1 else 1
                        OP("dve", "tensor_scalar", [b_S, b_mid], [b_junkD, b_cnt], out=junk8[:, 0:a], in0=S_row[:, 0:a], scalar1=bmid[:, 0:1],
                           scalar2=None, op0=ALU.is_ge, op1=ALU.add, accum_out=bcnt[:, 0:1])
                        OP("act", "activation", [b_S, b_mid], [b_junkA, b_sgn], out=junk8[:, a:n_i], in_=S_row[:, a:n_i], func=ACTF.Sign,
                           scale=-1.0, bias=bmid[:, 0:1], accum_out=bsgn[:, 0:1])
                        OP("dve", "scalar_tensor_tensor", [b_cnt, b_sgn], [b_u], out=bu[:, 0:1], in0=bcnt[:, 0:1], scalar=2.0, in1=bsgn[:, 0:1],
                           op0=ALU.mult, op1=ALU.subtract)
                        OP("dve", "tensor_scalar", [b_u, b_st], [b_u], out=bu[:, 1:2], in0=bu[:, 0:1], scalar1=float(2 * topk - n_act),
                           scalar2=bst[:, 8 + k:9 + k], op0=ALU.is_ge, op1=ALU.mult)
                        OP("dve", "scalar_tensor_tensor", [b_u, b_st, b_mid], [b_mid], out=bmid[:, dst:dst + 1], in0=bu[:, 1:2],
                           scalar=bst[:, 9 + k:10 + k], in1=bmid[:, 0:1], op0=ALU.subtract, op1=ALU.add)
                        yield
                    tile_ctx[i]["theta"] = (bmid[:, 1:2], b_mid)

                def A_P4(i):
                    t0 = i * 128
                    qTa, b_qTa = tile_ctx[i]["qTa"]
                    ytile, b_ytile = tile_ctx[i]["ytile"]
                    theta, b_theta = tile_ctx[i]["theta"]
                    pO = [ps.alloc(), ps.alloc()]

                    def pv(c, PT, b_PT):
                        for h in range(8):
                            b2, hh = divmod(h, 4)
                            MM(pO[b2][0][:, hh * 65:(hh + 1) * 65], PT[:, h * 128:(h + 1) * 128], vaug[:, c, :], (c == 0 and hh == 0), (c == i),
                               [b_PT, b_vaug], [pO[b2][1]], skip_group_check=True)

                    prev = None
                    for c in range(i + 1):
                        NM, b_NM = NM_r.next()
                        OP("dve", "tensor_scalar", [b_S, b_theta], [b_NM], out=NM[:], in0=S_row[:, c * 128:(c + 1) * 128], scalar1=theta,
                           scalar2=MASK_NEG, op0=ALU.is_lt, op1=ALU.mult)
                        PT, b_PT = PT_r.next()
                        for hf in range(2):
                            pL = ps.alloc()
                            MM(pL[0][:], kTa[0:65, c * 128:(c + 1) * 128], qTa[0:65, hf * 512:(hf + 1) * 512], True, False,
                               [b_kTa, b_qTa], [pL[1]])
                            MM(pL[0][:], NM[:], I4[:], False, True, [b_NM, b_I4], [pL[1]])
                            OP("act", "activation", [pL[1]], [b_PT], out=PT[:, hf * 512:(hf + 1) * 512], in_=pL[0][:], func=ACTF.Exp)
                            ps.free(pL)
                        if prev is not None:
                            pv(*prev)
                        prev = (c, PT, b_PT)
                    pv(*prev)
                    rden, b_rden = rden_r.next()
                    for b2 in range(2):
                        den = AP(pO[b2][0], 64, [[512, 128], [65, 4]])
                        OP("dve", "reciprocal", [pO[b2][1]], [b_rden], out=rden[:, b2 * 4:(b2 + 1) * 4], in_=den)
                        num = AP(pO[b2][0], 0, [[512, 128], [65, 4], [1, 64]])
                        rb = AP(rden, b2 * 4, [[8, 128], [1, 4], [0, 64]])
                        dsty = AP(ytile, 512 + b2 * 256, [[1024, 128], [64, 4], [1, 64]])
                        OP("dve", "tensor_tensor", [pO[b2][1], b_rden], [b_ytile], out=dsty, in0=num, in1=rb, op=ALU.mult)
                        ps.free(pO[b2])
                    P.dma("pool", Y_d[t0:t0 + 128, :], ytile[:], reads=[b_ytile], group=f"yst{i % 2}")
                    if debug:
                        yf, b_yf = xs_r.next()
                        OP("pool", "tensor_copy", [b_ytile], [b_yf], out=yf[:], in_=ytile[:])
                        P.dma("sp", dbg["y"][t0:t0 + 128, :], yf[:], reads=[b_yf], group="dbgy")

                def drive(*gens):
                    gens = list(gens)
                    while gens:
                        for g in list(gens):
                            try:
                                next(g)
                            except StopIteration:
                                gens.remove(g)

                drive(A_P1(0))
                A_P2(0)
                for i in range(NT):
                    if i + 1 < NT:
                        drive(A_P3(i), A_P1(i + 1))
                    else:
                        drive(A_P3(i))
                    A_P4(i)
                    if i + 1 < NT:
                        A_P2(i + 1)
                P.emit(E)
                P.barrier()
                stats["A"] = (len(P.all_ops), P.n_wait)

        if "B" in phases:
            with ExitStack() as ph:
                EP = ph.enter_context
                P = Prog(nc, "B")

                def T(name, shape, dt):
                    return EP(nc.sbuf_tensor(name + "B", list(shape), dt)), P.buf(name)

                def OP(eng, method, reads=(), writes=(), **kw):
                    return P.op(eng, lambda e: getattr(e, method)(**kw), reads, writes)

                def MM(out, lhsT, rhs, start, stop, reads, writes, **kw):
                    return P.op("pe", lambda e: e.matmul(out, lhsT=lhsT, rhs=rhs, start=start, stop=stop, **kw), reads, writes)

                ps = PsumPool(nc, P, EP)
                identF, b_identF = T("identF", [128, 128], F32)
                identB, b_identB = T("identB", [128, 128], BF16)
                g1B, b_g1B = T("g1B", [128, D], F32)
                b1B, b_b1B = T("b1B", [128, D], F32)
                nhalf, b_nhalf = T("nhalf", [128, 2], F32)
                P.dma("sp", identF[:], c_ident, writes=[b_identF], group="c", mode="all")
                P.dma("sp", g1B[:], AP(ln1g_d.tensor, 0, [[0, 128], [1, D]]), writes=[b_g1B], group="c", mode="all")
                P.dma("sp", b1B[:], AP(ln1b_d.tensor, 0, [[0, 128], [1, D]]), writes=[b_b1B], group="c", mode="all")
                OP("dve", "tensor_copy", [b_identF], [b_identB], out=identB[:], in_=identF[:])
                OP("dve", "memset", [], [b_nhalf], ap=nhalf[:], constant=-0.5)
                wG = EP(nc.sbuf_tensor("wG", [128, KC, 2048], BF16))
                wBr = EP(nc.sbuf_tensor("wBr", [128, KC, D], BF16))
                wO = EP(nc.sbuf_tensor("wO", [128, KC, D], BF16))
                b_wG = [P.buf(f"wG{k}") for k in range(KC)]
                b_wBr = [P.buf(f"wBr{k}") for k in range(KC)]
                b_wO = [P.buf(f"wO{k}") for k in range(KC)]
                for kc in range(KC):
                    P.dma("pool", wG[:, kc, :], w_in_d[kc * 128:(kc + 1) * 128, NCOLA:NCOLA + 2048], writes=[b_wG[kc]], group=f"wG{kc}")
                    P.dma("pool", wBr[:, kc, :], w_br_d[kc * 128:(kc + 1) * 128, :], writes=[b_wBr[kc]], group=f"wBr{kc}")
                    P.dma("pool", wO[:, kc, :], w_o_d[kc * 128:(kc + 1) * 128, :], writes=[b_wO[kc]], group=f"wO{kc}")
                xs_r = Rot(nc, P, EP, "xs", [128, D], F32, 3)
                ys_r = Rot(nc, P, EP, "ys", [128, D], BF16, 3)
                xT_r = Rot(nc, P, EP, "xT", [128, KC, 128], BF16, 2)
                yT_r = Rot(nc, P, EP, "yT", [128, KC, 128], BF16, 2)
                sg_r = Rot(nc, P, EP, "sg", [128, 512], F32, 3)
                m0_r = Rot(nc, P, EP, "m0", [128, 512], F32, 3)
                m1_r = Rot(nc, P, EP, "m1", [128, 512], F32, 3)
                mT_r = Rot(nc, P, EP, "mT", [128, KC, 128], BF16, 2)
                yres_r = Rot(nc, P, EP, "yres", [128, D], F32, 2)
                x1_r = Rot(nc, P, EP, "x1", [128, D], F32, 2)
                st_r = Rot(nc, P, EP, "st", [128, 16], F32, 2)

                def layer_norm(src, b_src, dst, b_dst, gB, b_gB, bB, b_bB, st, b_st, nh, b_nh):
                    for q in range(2):
                        OP("dve", "bn_stats", [b_src], [b_st], out=st[:, q * 6:(q + 1) * 6], in_=src[:, q * 512:(q + 1) * 512])
                    OP("dve", "bn_aggr", [b_st], [b_st], out=st[:, 12:14], in_=st[:, 0:12])
                    OP("dve", "tensor_scalar", [b_st], [b_st], out=st[:, 14:15], in0=st[:, 13:14], scalar1=LN_EPS, scalar2=None, op0=ALU.add)
                    OP("pool", "tensor_tensor", [b_st, b_nh], [b_st], out=st[:, 15:16], in0=st[:, 14:15], in1=nh[:, 0:1], op=ALU.pow)
                    OP("dve", "tensor_scalar", [b_src, b_st], [b_dst], out=dst[:], in0=src[:], scalar1=st[:, 12:13], scalar2=st[:, 15:16],
                       op0=ALU.subtract, op1=ALU.mult)
                    OP("pool", "tensor_tensor", [b_dst, b_gB], [b_dst], out=dst[:], in0=dst[:], in1=gB[:], op=ALU.mult)
                    OP("pool", "tensor_tensor", [b_dst, b_bB], [b_dst], out=dst[:], in0=dst[:], in1=bB[:], op=ALU.add)

                def B_tile(i):
                    t0 = i * 128
                    xs, b_xs = xs_r.next()
                    ys, b_ys = ys_r.next()
                    P.dma("sp", xs[:], x_d[t0:t0 + 128, :], writes=[b_xs], group=f"xs{i % 3}")
                    P.dma("sp", ys[:], Y_d[t0:t0 + 128, :], writes=[b_ys], group=f"ys{i % 3}")
                    xT, b_xT = xT_r.next()
                    yT, b_yT = yT_r.next()
                    for half in range(2):
                        pb = ps.alloc()
                        for q in range(4):
                            kc = half * 4 + q
                            OP("pe", "transpose", [b_xs, b_identF], [pb[1]], out=pb[0][:, q * 128:(q + 1) * 128],
                               in_=xs[:, kc * 128:(kc + 1) * 128], identity=identF[:])
                        OP("act", "activation", [pb[1]], [b_xT], out=AP(xT, half * 512, [[1024, 128], [1, 512]]), in_=pb[0][:], func=ACTF.Copy)
                        ps.free(pb)
                        pb = ps.alloc()
                        for q in range(4):
                            kc = half * 4 + q
                            MM(pb[0][:, q * 128:(q + 1) * 128], ys[:, kc * 128:(kc + 1) * 128], identB[:], True, True, [b_ys, b_identB], [pb[1]])
                        OP("dve", "tensor_copy", [pb[1]], [b_yT], out=AP(yT, half * 512, [[1024, 128], [1, 512]]), in_=pb[0][:])
                        ps.free(pb)
                    mT, b_mT = mT_r.next()
                    for half in range(2):
                        held = None
                        for n in range(2):
                            pg = ps.alloc()
                            pp = ps.alloc()
                            for q in range(4):
                                dc = half * 4 + q
                                c0 = n * 1024 + dc * 128
                                for kc in range(KC):
                                    MM(pg[0][:, q * 128:(q + 1) * 128], wG[:, kc, c0:c0 + 128], xT[:, kc, :], kc == 0, kc == KC - 1,
                                       [b_wG[kc], b_xT], [pg[1]])
                                for cc in range(4):
                                    MM(pp[0][:, q * 128:(q + 1) * 128], wBr[:, n * 4 + cc, dc * 128:(dc + 1) * 128], yT[:, n * 4 + cc, :], cc == 0, cc == 3,
                                       [b_wBr[n * 4 + cc], b_yT], [pp[1]])
                            sg, b_sg = sg_r.next()
                            OP("act", "activation", [pg[1]], [b_sg], out=sg[:], in_=pg[0][:], func=ACTF.Sigmoid)
                            ps.free(pg)
                            if n == 0:
                                m0, b_m0 = m0_r.next()
                                OP("dve", "tensor_tensor", [pp[1], b_sg], [b_m0], out=m0[:], in0=pp[0][:], in1=sg[:], op=ALU.mult)
                                held = (m0, b_m0)
                            else:
                                m1, b_m1 = m1_r.next()
                                OP("dve", "tensor_tensor", [pp[1], b_sg], [b_m1], out=m1[:], in0=pp[0][:], in1=sg[:], op=ALU.mult)
                                OP("pool", "tensor_tensor", [b_m1, held[1]], [b_mT], out=AP(mT, half * 512, [[1024, 128], [1, 512]]), in0=m1[:], in1=held[0][:],
                                   op=ALU.add)
                            ps.free(pp)
                    yres, b_yres = yres_r.next()
                    for nb in range(2):
                        pm = ps.alloc()
                        for dc in range(KC):
                            MM(pm[0][:], mT[:, dc, :], wO[:, dc, nb * 512:(nb + 1) * 512], dc == 0, dc == KC - 1, [b_mT, b_wO[dc]], [pm[1]])
                        OP("dve", "scalar_tensor_tensor", [b_xs, pm[1]], [b_yres], out=yres[:, nb * 512:(nb + 1) * 512], in0=xs[:, nb * 512:(nb + 1) * 512],
                           scalar=ALPHA, in1=pm[0][:], op0=ALU.mult, op1=ALU.add)
                        ps.free(pm)
                    x1, b_x1 = x1_r.next()
                    st, b_st = st_r.next()
                    layer_norm(yres, b_yres, x1, b_x1, g1B, b_g1B, b1B, b_b1B, st, b_st, nhalf, b_nhalf)
                    P.dma("pool", X1_d[t0:t0 + 128, :], x1[:], reads=[b_x1], group=f"x1st{i % 2}")
                    if debug:
                        P.dma("sp", dbg["x1"][t0:t0 + 128, :], x1[:], reads=[b_x1], group="dbgx1")

                for i in range(NT if "B" in phases else 0):
                    B_tile(i)
                P.emit(E)
                P.barrier()
                stats["B"] = (len(P.all_ops), P.n_wait)

        if "C" in phases:
            with ExitStack() as ph:
                EP = ph.enter_context
                P = Prog(nc, "C")
                ST = 256 if L % 256 == 0 else 128
                NS = L // ST
                TPS = ST // 128

                def T(name, shape, dt):
                    return EP(nc.sbuf_tensor(name + "C", list(shape), dt)), P.buf(name)

                def OP(eng, method, reads=(), writes=(), **kw):
                    return P.op(eng, lambda e: getattr(e, method)(**kw), reads, writes)

                def MM(out, lhsT, rhs, start, stop, reads, writes, **kw):
                    return P.op("pe", lambda e: e.matmul(out, lhsT=lhsT, rhs=rhs, start=start, stop=stop, **kw), reads, writes)

                ps = PsumPool(nc, P, EP)
                identF, b_identF = T("identF", [128, 128], F32)
                g2B, b_g2B = T("g2B", [128, D], F32)
                b2B, b_b2B = T("b2B", [128, D], F32)
                nhalf, b_nhalf = T("nhalf", [128, 2], F32)
                cw, b_cw = T("cw", [128, 3, 2 * NFC], F32)
                cb, b_cb = T("cb", [128, 2 * NFC], F32)
                halo, b_halo = T("halo", [128, 2 * NFC, 2], F32)
                P.dma("sp", identF[:], c_ident, writes=[b_identF], group="c", mode="all")
                P.dma("sp", g2B[:], AP(ln2g_d.tensor, 0, [[0, 128], [1, D]]), writes=[b_g2B], group="c", mode="all")
                P.dma("sp", b2B[:], AP(ln2b_d.tensor, 0, [[0, 128], [1, D]]), writes=[b_b2B], group="c", mode="all")
                cwraw, _unused = T("cwraw", [2 * NFC, 4, 128], F32)
                b_cwr = [P.buf(f"cwraw{j}") for j in range(4)]
                for j in range(3):
                    P.dma("sp", cwraw[:, j, :], AP(cw_d.tensor, j * 2 * D_FF, [[128, 2 * NFC], [1, 128]]), writes=[b_cwr[j]], group="c", mode="all")
                P.dma("sp", cwraw[:, 3, :], AP(cb_d.tensor, 0, [[128, 2 * NFC], [1, 128]]), writes=[b_cwr[3]], group="c", mode="all")
                pcw = ps.alloc()
                for j in range(4):
                    OP("pe", "transpose", [b_cwr[j], b_identF], [pcw[1]], out=pcw[0][:, j * 2 * NFC:(j + 1) * 2 * NFC], in_=cwraw[:, j, :],
                       identity=identF[0:2 * NFC, 0:2 * NFC])
                OP("act", "activation", [pcw[1]], [b_cw], out=AP(cw, 0, [[6 * NFC, 128], [1, 6 * NFC]]), in_=pcw[0][:, 0:6 * NFC], func=ACTF.Copy)
                OP("act", "activation", [pcw[1]], [b_cb], out=cb[:], in_=pcw[0][:, 6 * NFC:8 * NFC], func=ACTF.Copy)
                ps.free(pcw)
                OP("dve", "memset", [], [b_nhalf], ap=nhalf[:], constant=-0.5)
                OP("dve", "memset", [], [b_halo], ap=halo[:], constant=0.0)
                wU = EP(nc.sbuf_tensor("wU", [128, KC, 2 * D_FF], BF16))
                wDn = EP(nc.sbuf_tensor("wDn", [128, NFC, D], BF16))
                b_wU = [P.buf(f"wU{k}") for k in range(KC)]
                b_wDn = [P.buf(f"wDn{k}") for k in range(NFC)]
                for kc in range(KC):
                    P.dma("pool", wU[:, kc, :], w_up_d[kc * 128:(kc + 1) * 128, :], writes=[b_wU[kc]], group=f"wU{kc}")
                for j in range(NFC):
                    P.dma("pool", wDn[:, j, :], w_dn_d[j * 128:(j + 1) * 128, :], writes=[b_wDn[j]], group=f"wD{j}")
                xs_r = Rot(nc, P, EP, "xs", [128, D], F32, 2 * TPS)
                xT_r = Rot(nc, P, EP, "xT", [128, KC, ST], BF16, 2)
                upb_r = Rot(nc, P, EP, "upb", [128, ST + 2], F32, 4)
                u_r = Rot(nc, P, EP, "u", [128, ST], F32, 4)
                sl_r = Rot(nc, P, EP, "sl", [128, ST], F32, 2)
                gT_r = Rot(nc, P, EP, "gT", [128, NFC, ST], BF16, 1)
                yres_r = Rot(nc, P, EP, "yres", [128, D], F32, 1)
                o_r = Rot(nc, P, EP, "o", [128, D], F32, 2)
                st_r = Rot(nc, P, EP, "st", [128, 16], F32, 2)

                def layer_norm(src, b_src, dst, b_dst, gB, b_gB, bB, b_bB, st, b_st, nh, b_nh):
                    for q in range(2):
                        OP("dve", "bn_stats", [b_src], [b_st], out=st[:, q * 6:(q + 1) * 6], in_=src[:, q * 512:(q + 1) * 512])
                    OP("dve", "bn_aggr", [b_st], [b_st], out=st[:, 12:14], in_=st[:, 0:12])
                    OP("dve", "tensor_scalar", [b_st], [b_st], out=st[:, 14:15], in0=st[:, 13:14], scalar1=LN_EPS, scalar2=None, op0=ALU.add)
                    OP("pool", "tensor_tensor", [b_st, b_nh], [b_st], out=st[:, 15:16], in0=st[:, 14:15], in1=nh[:, 0:1], op=ALU.pow)
                    OP("dve", "tensor_scalar", [b_src, b_st], [b_dst], out=dst[:], in0=src[:], scalar1=st[:, 12:13], scalar2=st[:, 15:16],
                       op0=ALU.subtract, op1=ALU.mult)
                    OP("pool", "tensor_tensor", [b_dst, b_gB], [b_dst], out=dst[:], in0=dst[:], in1=gB[:], op=ALU.mult)
                    OP("pool", "tensor_tensor", [b_dst, b_bB], [b_dst], out=dst[:], in0=dst[:], in1=bB[:], op=ALU.add)

                def conv_chunk(ch, xT, b_xT):
                    pu = ps.alloc()
                    for kc in range(KC):
                        MM(pu[0][:, 0:ST], wU[:, kc, ch * 128:(ch + 1) * 128], xT[:, kc, :], kc == 0, kc == KC - 1, [b_wU[kc], b_xT], [pu[1]])
                    upb, b_upb = upb_r.next()
                    OP("pool", "tensor_copy", [b_halo], [b_upb], out=upb[:, 0:2], in_=halo[:, ch, :])
                    OP("act", "activation", [pu[1]], [b_upb], out=upb[:, 2:ST + 2], in_=pu[0][:, 0:ST], func=ACTF.Copy)
                    ps.free(pu)
                    OP("pool", "tensor_copy", [b_upb], [b_halo], out=halo[:, ch, :], in_=upb[:, ST:ST + 2])
                    u, b_u = u_r.next()
                    OP("act", "activation", [b_upb, b_cw, b_cb], [b_u], out=u[:], in_=upb[:, 2:ST + 2], func=ACTF.Identity,
                       scale=cw[:, 2, ch:ch + 1], bias=cb[:, ch:ch + 1])
                    OP("dve", "scalar_tensor_tensor", [b_upb, b_cw, b_u], [b_u], out=u[:], in0=upb[:, 1:ST + 1], scalar=cw[:, 1, ch:ch + 1], in1=u[:],
                       op0=ALU.mult, op1=ALU.add)
                    OP("dve", "scalar_tensor_tensor", [b_upb, b_cw, b_u], [b_u], out=u[:], in0=upb[:, 0:ST], scalar=cw[:, 0, ch:ch + 1], in1=u[:],
                       op0=ALU.mult, op1=ALU.add)
                    return u, b_u

                def C_super(s):
                    t0 = s * ST
                    xtiles = []
                    xT, b_xT = xT_r.next()
                    for tt in range(TPS):
                        xs, b_xs = xs_r.next()
                        P.dma("sp", xs[:], X1_d[t0 + tt * 128:t0 + (tt + 1) * 128, :], writes=[b_xs], group=f"xs{(s * TPS + tt) % (2 * TPS)}")
                        xtiles.append((xs, b_xs))
                        for half in range(2):
                            pb = ps.alloc()
                            for q in range(4):
                                kc = half * 4 + q
                                OP("pe", "transpose", [b_xs, b_identF], [pb[1]], out=pb[0][:, q * 128:(q + 1) * 128],
                                   in_=xs[:, kc * 128:(kc + 1) * 128], identity=identF[:])
                            dst = AP(xT, half * 4 * ST + tt * 128, [[KC * ST, 128], [ST, 4], [1, 128]])
                            src = AP(pb[0], 0, [[512, 128], [128, 4], [1, 128]])
                            if half == 0:
                                OP("act", "activation", [pb[1]], [b_xT], out=dst, in_=src, func=ACTF.Copy)
                            else:
                                OP("dve", "tensor_copy", [pb[1]], [b_xT], out=dst, in_=src)
                            ps.free(pb)
                    gT, b_gT = gT_r.next()
                    for j in range(NFC):
                        ug, b_ug = conv_chunk(j, xT, b_xT)
                        uv, b_uv = conv_chunk(NFC + j, xT, b_xT)
                        sl, b_sl = sl_r.next()
                        OP("act", "activation", [b_ug], [b_sl], out=sl[:], in_=ug[:], func=ACTF.Silu)
                        OP("dve", "tensor_tensor", [b_sl, b_uv], [b_gT], out=gT[:, j, :], in0=sl[:], in1=uv[:], op=ALU.mult)
                    for tt in range(TPS):
                        xs, b_xs = xtiles[tt]
                        yres, b_yres = yres_r.next()
                        for nb in range(2):
                            pm = ps.alloc()
                            for j in range(NFC):
                                MM(pm[0][:], gT[:, j, tt * 128:(tt + 1) * 128], wDn[:, j, nb * 512:(nb + 1) * 512], j == 0, j == NFC - 1,
                                   [b_gT, b_wDn[j]], [pm[1]])
                            OP("dve", "scalar_tensor_tensor", [b_xs, pm[1]], [b_yres], out=yres[:, nb * 512:(nb + 1) * 512],
                               in0=xs[:, nb * 512:(nb + 1) * 512], scalar=ALPHA, in1=pm[0][:], op0=ALU.mult, op1=ALU.add)
                            ps.free(pm)
                        ot, b_ot = o_r.next()
                        st, b_st = st_r.next()
                        layer_norm(yres, b_yres, ot, b_ot, g2B, b_g2B, b2B, b_b2B, st, b_st, nhalf, b_nhalf)
                        P.dma("pool", out_d[t0 + tt * 128:t0 + (tt + 1) * 128, :], ot[:], reads=[b_ot], group=f"ost{(s * TPS + tt) % 2}")

                for s in range(NS if "C" in phases else 0):
                    C_super(s)
                P.emit(E)
                P.barrier(("sp",))
                stats["C"] = (len(P.all_ops), P.n_wait)
    return nc, stats


def host_consts(L):
    idx = np.arange(128)
    ident = np.eye(128, dtype=np.float32)
    le = (idx[:, None] <= idx[None, :])
    uneg = np.where(le, -1.0 / 16.0, 0.0).astype(np.float32)
    lneg = np.where(idx[:, None] > idx[None, :], -1.0 / 16.0, 0.0).astype(np.float32)
    u01 = le.astype(np.float32)
    cm = np.where(idx[None, :] <= idx[:, None], 0.0, NEG_BIG).astype(np.float32)
    pos = np.arange(L, dtype=np.float32)

    def tab(rot):
        half = rot // 2
        inv = (np.float32(500000.0) ** (-np.arange(half, dtype=np.float32) * np.float32(2.0) / np.float32(rot))).astype(np.float32)
        ang = (pos[:, None] * inv[None, :]).astype(np.float32)
        c = np.cos(ang).astype(np.float32)
        s = np.sin(ang).astype(np.float32)
        return np.concatenate([c, c, -s, s], axis=1)

    rope = np.concatenate([tab(16), tab(8)], axis=1).astype(np.float32)
    pow2 = np.tile((2.0 ** -(np.arange(16, dtype=np.float32) + 1.0))[None, :], (128, 1)).astype(np.float32)
    return dict(c_ident=ident, c_uneg=uneg, c_lneg=lneg, c_u01=u01, c_cm=cm, c_rope=rope, c_pow2=pow2)


def make_in_maps(inputs, L, n_cores):
    f = lambda a: np.ascontiguousarray(np.asarray(a, dtype=np.float32))
    consts = host_consts(L)
    shared = dict(
        w_in=f(np.asarray(inputs["w_in"])[0][:, W_IN_PERM]),
        w_a2=f(inputs["w_gla_a2"][0]),
        b_a=f(np.asarray(inputs["b_gla_a"])[0][None, :]),
        gla_gain=f(np.asarray(inputs["gla_norm_gain"])[0].reshape(1, 512)),
        w_br=f(np.asarray(inputs["w_branch"])[0].reshape(1024, D)),
        w_o=f(inputs["w_o"][0]),
        ln1g=f(np.asarray(inputs["ln1_gain"])[0][None, :]),
        ln1b=f(np.asarray(inputs["ln1_bias"])[0][None, :]),
        w_up=f(inputs["w_up"][0]),
        conv_w=f(inputs["conv_w"][0]),
        conv_b=f(np.asarray(inputs["conv_b"])[0][None, :]),
        w_down=f(inputs["w_down"][0]),
        ln2g=f(np.asarray(inputs["ln2_gain"])[0][None, :]),
        ln2b=f(np.asarray(inputs["ln2_bias"])[0][None, :]),
        **consts,
    )
    x = np.asarray(inputs["x"], dtype=np.float32)
    return [dict(shared, x=np.ascontiguousarray(x[b])) for b in range(n_cores)]


_CACHE = {}


def kernel(**inputs):
    x = np.asarray(inputs["x"])
    B, L, _ = x.shape
    topk = min(256, L // 4)
    key = (L, topk)
    if key not in _CACHE:
        _CACHE[key] = build(L, topk)[0]
    nc = _CACHE[key]
    in_maps = make_in_maps(inputs, L, B)
    res = run_bass_kernel_spmd(nc, in_maps, core_ids=list(range(B)))
    return np.stack([np.asarray(r["out"], dtype=np.float32) for r in res.results], axis=0)
```
